# Optimizing a Trainium2 kernel written in Bass

```python
import math
import jax, jax.numpy as jnp
from jax import lax
import numpy as np

D_MODEL = 1024
BATCH = 4
SEQ = 8192
DEPTH = 1

ATT_WIDTH = D_MODEL // 2
HEAD_DIM = 64
N_ATT_HEADS = ATT_WIDTH // HEAD_DIM
SSM_WIDTH = D_MODEL - ATT_WIDTH
SSM_GROUP = 16
N_SSM_GROUPS = SSM_WIDTH // SSM_GROUP
SSM_STATE = 64
MIX_WIDTH = ATT_WIDTH + SSM_WIDTH
DILATED_PATTERNS = ((128, 1), (512, 4), (2048, 16))
ATT_BLOCK = 128
N_BUCKETS = 32
MAX_DISTANCE = 2048
N_EXPERTS = 256
TOP_K = 8
N_EXPERT_GROUPS = 8
TOPK_GROUPS = 4
EXPERT_FF = 256
SHARED_FF = 256
ROUTED_SCALE = 2.5
MOE_BLOCK = 128
ALPHA = (2 * DEPTH) ** 0.25
BETA = (8 * DEPTH) ** -0.25
EPS = 1e-5
NEG_INF = -1e30

kernel_name = 'hybrid_dilated_s5_moe_block'


def layer_norm(x, g, b):
    xf = x.astype(jnp.float32)
    mu = jnp.mean(xf, -1, keepdims=True)
    var = jnp.mean(jnp.square(xf - mu), -1, keepdims=True)
    return ((xf - mu) * lax.rsqrt(var + EPS) * g + b).astype(x.dtype)


def rms_norm(x, g):
    xf = x.astype(jnp.float32)
    return (xf * lax.rsqrt(jnp.mean(jnp.square(xf), -1, keepdims=True) + EPS) * g).astype(x.dtype)


def t5_bucket(dist):
    exact = N_BUCKETS // 2
    large = exact + (jnp.log(jnp.maximum(dist, 1).astype(jnp.float32) / exact)
                     / math.log(MAX_DISTANCE / exact) * (N_BUCKETS - exact)).astype(jnp.int32)
    return jnp.where(dist < exact, dist, jnp.minimum(large, N_BUCKETS - 1))


def dilated_window_pattern(q, k, v, rel_bias, window, dilation):
    bsz, seq, heads, hd = q.shape
    n_strided = seq // dilation
    n_blocks = -(-n_strided // ATT_BLOCK)
    padded = n_blocks * ATT_BLOCK

    def strided_blocks(t):
        t = t.reshape(bsz, n_strided, dilation, heads, hd)
        t = jnp.pad(t, ((0, 0), (0, padded - n_strided), (0, 0), (0, 0), (0, 0)))
        return t.reshape(bsz, n_blocks, ATT_BLOCK, dilation, heads, hd)

    def with_previous(t):
        prev = jnp.pad(t[:, :-1], ((0, 0), (1, 0), (0, 0), (0, 0), (0, 0), (0, 0)))
        return jnp.concatenate([prev, t], axis=2)

    qb = strided_blocks(q)
    kb = with_previous(strided_blocks(k))
    vb = with_previous(strided_blocks(v))
    qi = jnp.arange(ATT_BLOCK)[:, None]
    ki = jnp.arange(2 * ATT_BLOCK)[None, :]
    rel = qi + ATT_BLOCK - ki
    band = (rel >= 0) & (rel <= window // dilation)
    key_ok = (jnp.arange(n_blocks)[:, None] * ATT_BLOCK - ATT_BLOCK + ki) >= 0
    mask = band[None] & key_ok[:, None, :]
    bias = jnp.transpose(rel_bias[t5_bucket(jnp.maximum(rel, 0) * dilation)], (2, 0, 1)).astype(jnp.float32)
    s = jnp.einsum('bnqrhd,bnkrhd->bnrhqk', qb, kb).astype(jnp.float32) * HEAD_DIM ** -0.5 + bias
    s = jnp.where(mask[None, :, None, None], s, NEG_INF)
    m = jnp.max(s, -1, keepdims=True)
    p = jnp.exp(s - m)
    l = jnp.sum(p, -1)
    o = jnp.einsum('bnrhqk,bnkrhd->bnqrhd', p, vb.astype(jnp.float32))
    l_t = jnp.transpose(l, (0, 1, 4, 2, 3))
    o = o / l_t[..., None]
    lse = jnp.transpose(m[..., 0], (0, 1, 4, 2, 3)) + jnp.log(l_t)
    o = o.reshape(bsz, padded, dilation, heads, hd)[:, :n_strided].reshape(bsz, seq, heads, hd)
    lse = lse.reshape(bsz, padded, dilation, heads)[:, :n_strided].reshape(bsz, seq, heads)
    return o, lse


def dilated_attention(q, k, v, rel_bias):
    outs, lses = zip(*[dilated_window_pattern(q, k, v, rel_bias, w, d) for w, d in DILATED_PATTERNS])
    wts = jax.nn.softmax(jnp.stack(lses), axis=0)
    return jnp.sum(wts[..., None] * jnp.stack(outs), axis=0)


def s5_layer(u, a_re, a_im, b_re, b_im, c_re, c_im, d_skip, log_dt, w_glu, b_glu):
    bsz, seq, _ = u.shape
    f32 = jnp.float32
    uf = u.astype(f32).reshape(bsz, seq, N_SSM_GROUPS, SSM_GROUP)
    a = lax.complex(a_re.astype(f32), a_im.astype(f32))
    dt = jnp.exp(log_dt.astype(f32))[:, None]
    a_bar = jnp.exp(dt * a)
    b_bar = ((a_bar - 1.0) / a)[:, :, None] * lax.complex(b_re.astype(f32), b_im.astype(f32))
    c_mat = lax.complex(c_re.astype(f32), c_im.astype(f32))
    bu = jnp.einsum('bsgc,gpc->bsgp', uf.astype(jnp.complex64), b_bar)
    decay = jnp.broadcast_to(a_bar, bu.shape)

    def combine(e1, e2):
        a1, h1 = e1
        a2, h2 = e2
        return a1 * a2, a2 * h1 + h2

    _, state = lax.associative_scan(combine, (decay, bu), axis=1)
    y = jnp.einsum('bsgp,gcp->bsgc', state, c_mat).real + d_skip.astype(f32) * uf
    y = jax.nn.gelu(y.reshape(bsz, seq, SSM_WIDTH))
    y = y * jax.nn.sigmoid(y @ w_glu.astype(f32) + b_glu.astype(f32))
    return y.astype(u.dtype)


def parallel_mixer(h, w_in, rel_bias, a_re, a_im, b_re, b_im, c_re, c_im, d_skip, log_dt,
                   w_glu, b_glu, g_att, g_ssm, w_out):
    bsz, seq, _ = h.shape
    proj = h @ w_in
    q, k, v, u = jnp.split(proj, [ATT_WIDTH, 2 * ATT_WIDTH, 3 * ATT_WIDTH], axis=-1)
    heads = lambda t: t.reshape(bsz, seq, N_ATT_HEADS, HEAD_DIM)
    att = dilated_attention(heads(q), heads(k), heads(v), rel_bias).reshape(bsz, seq, ATT_WIDTH).astype(h.dtype)
    ssm = s5_layer(u, a_re, a_im, b_re, b_im, c_re, c_im, d_skip, log_dt, w_glu, b_glu)
    merged = jnp.concatenate([rms_norm(att, g_att), rms_norm(ssm, g_ssm)], axis=-1)
    return merged @ w_out


def moe_ffn(h, w_router, router_bias, w_e_gate, w_e_up, w_e_down, w_s_gate, w_s_up, w_s_down):
    bsz, seq, d = h.shape
    n_tok = bsz * seq
    xt = h.reshape(n_tok, d)
    scores = jax.nn.sigmoid((xt @ w_router).astype(jnp.float32))
    biased = scores + router_bias.astype(jnp.float32)
    grouped = biased.reshape(n_tok, N_EXPERT_GROUPS, N_EXPERTS // N_EXPERT_GROUPS)
    group_score = jnp.sum(lax.top_k(grouped, 2)[0], -1)
    _, top_groups = lax.top_k(group_score, TOPK_GROUPS)
    group_ok = jnp.any(top_groups[:, :, None] == jnp.arange(N_EXPERT_GROUPS)[None, None, :], axis=1)
    masked = jnp.where(group_ok[:, :, None], grouped, -jnp.inf).reshape(n_tok, N_EXPERTS)
    _, expert_idx = lax.top_k(masked, TOP_K)
    gate = jnp.take_along_axis(scores, expert_idx, axis=1)
    gate = gate / jnp.sum(gate, -1, keepdims=True) * ROUTED_SCALE
    n_assign = n_tok * TOP_K
    flat_e = expert_idx.reshape(-1)
    order = jnp.argsort(flat_e)
    sorted_e = flat_e[order]
    sorted_tok = (order // TOP_K).astype(jnp.int32)
    sorted_gate = gate.reshape(-1)[order]
    counts = jnp.bincount(flat_e, length=N_EXPERTS)
    padded_counts = (counts + MOE_BLOCK - 1) // MOE_BLOCK * MOE_BLOCK
    padded_end = jnp.cumsum(padded_counts)
    padded_start = padded_end - padded_counts
    start = jnp.cumsum(counts) - counts
    dest = padded_start[sorted_e] + jnp.arange(n_assign) - start[sorted_e]
    n_blocks = -(-n_assign // MOE_BLOCK) + N_EXPERTS
    n_rows = n_blocks * MOE_BLOCK
    row_tok = jnp.full((n_rows,), n_tok, jnp.int32).at[dest].set(sorted_tok)
    row_gate = jnp.zeros((n_rows,), jnp.float32).at[dest].set(sorted_gate)
    block_expert = jnp.minimum(jnp.searchsorted(padded_end, jnp.arange(n_blocks) * MOE_BLOCK, side='right'),
                               N_EXPERTS - 1)
    x_pad = jnp.concatenate([xt, jnp.zeros((1, d), xt.dtype)], axis=0)

    def expert_block(args):
        rows, g, e = args
        xb = x_pad[rows]
        hid = jax.nn.silu(xb @ w_e_gate[e]) * (xb @ w_e_up[e])
        return (hid @ w_e_down[e]) * g[:, None].astype(xb.dtype)

    routed = lax.map(expert_block, (row_tok.reshape(n_blocks, MOE_BLOCK),
                                    row_gate.reshape(n_blocks, MOE_BLOCK), block_expert))
    routed = jax.ops.segment_sum(routed.reshape(n_rows, d), row_tok, num_segments=n_tok + 1)[:n_tok]
    shared = (jax.nn.silu(xt @ w_s_gate) * (xt @ w_s_up)) @ w_s_down
    return (routed + shared).reshape(bsz, seq, d)


def setup_inputs(seed: int = 0) -> dict:
    key = jax.random.key(seed)
    ks = jax.random.split(key, 32)
    f32 = jnp.float32

    def nrm(k, shape, scale):
        return jax.random.normal(k, shape, f32) * scale

    L, G, P, GC = DEPTH, N_SSM_GROUPS, SSM_STATE, SSM_GROUP
    n_idx = jnp.arange(P, dtype=f32)
    return {
        'x': nrm(ks[0], (BATCH, SEQ, D_MODEL), 1.0),
        'c': nrm(ks[1], (BATCH, D_MODEL), 1.0),
        'rel_bias': nrm(ks[2], (N_BUCKETS, N_ATT_HEADS), 0.5),
        'w_ada': nrm(ks[3], (L, D_MODEL, 6 * D_MODEL), 0.5 * D_MODEL ** -0.5),
        'b_ada': nrm(ks[4], (L, 6 * D_MODEL), 0.02),
        'w_in': nrm(ks[5], (L, D_MODEL, 3 * ATT_WIDTH + SSM_WIDTH), D_MODEL ** -0.5),
        'ssm_a_re': -0.5 + nrm(ks[6], (L, G, P), 0.01),
        'ssm_a_im': math.pi * n_idx + nrm(ks[7], (L, G, P), 0.01),
        'ssm_b_re': nrm(ks[8], (L, G, P, GC), (2 * GC) ** -0.5),
        'ssm_b_im': nrm(ks[9], (L, G, P, GC), (2 * GC) ** -0.5),
        'ssm_c_re': nrm(ks[10], (L, G, GC, P), (2 * P) ** -0.5),
        'ssm_c_im': nrm(ks[11], (L, G, GC, P), (2 * P) ** -0.5),
        'ssm_d': nrm(ks[12], (L, G, GC), 0.5),
        'ssm_log_dt': jax.random.uniform(ks[13], (L, G), f32, math.log(0.001), math.log(0.1)),
        'w_glu': nrm(ks[14], (L, SSM_WIDTH, SSM_WIDTH), SSM_WIDTH ** -0.5),
        'b_glu': nrm(ks[15], (L, SSM_WIDTH), 0.02),
        'g_att': 1.0 + nrm(ks[16], (L, ATT_WIDTH), 0.02),
        'g_ssm': 1.0 + nrm(ks[17], (L, SSM_WIDTH), 0.02),
        'w_out': nrm(ks[18], (L, MIX_WIDTH, D_MODEL), BETA * MIX_WIDTH ** -0.5),
        'ln1_g': 1.0 + nrm(ks[19], (L, D_MODEL), 0.02),
        'ln1_b': nrm(ks[20], (L, D_MODEL), 0.02),
        'w_router': nrm(ks[21], (L, D_MODEL, N_EXPERTS), D_MODEL ** -0.5),
        'router_bias': nrm(ks[22], (L, N_EXPERTS), 0.01),
        'w_e_gate': nrm(ks[23], (L, N_EXPERTS, D_MODEL, EXPERT_FF), D_MODEL ** -0.5),
        'w_e_up': nrm(ks[24], (L, N_EXPERTS, D_MODEL, EXPERT_FF), D_MODEL ** -0.5),
        'w_e_down': nrm(ks[25], (L, N_EXPERTS, EXPERT_FF, D_MODEL), BETA * EXPERT_FF ** -0.5),
        'w_s_gate': nrm(ks[26], (L, D_MODEL, SHARED_FF), D_MODEL ** -0.5),
        'w_s_up': nrm(ks[27], (L, D_MODEL, SHARED_FF), D_MODEL ** -0.5),
        'w_s_down': nrm(ks[28], (L, SHARED_FF, D_MODEL), BETA * SHARED_FF ** -0.5),
        'ln2_g': 1.0 + nrm(ks[29], (L, D_MODEL), 0.02),
        'ln2_b': nrm(ks[30], (L, D_MODEL), 0.02),
    }


def reference(x, c, rel_bias, w_ada, b_ada, w_in, ssm_a_re, ssm_a_im, ssm_b_re, ssm_b_im,
              ssm_c_re, ssm_c_im, ssm_d, ssm_log_dt, w_glu, b_glu, g_att, g_ssm, w_out,
              ln1_g, ln1_b, w_router, router_bias, w_e_gate, w_e_up, w_e_down,
              w_s_gate, w_s_up, w_s_down, ln2_g, ln2_b):
    for layer in range(DEPTH):
        ada = jax.nn.silu(c) @ w_ada[layer] + b_ada[layer]
        shift1, scale1, gate1, shift2, scale2, gate2 = jnp.split(ada[:, None, :], 6, axis=-1)
        h = x * (1.0 + scale1) + shift1
        mix = parallel_mixer(h, w_in[layer], rel_bias, ssm_a_re[layer], ssm_a_im[layer],
                             ssm_b_re[layer], ssm_b_im[layer], ssm_c_re[layer], ssm_c_im[layer],
                             ssm_d[layer], ssm_log_dt[layer], w_glu[layer], b_glu[layer],
                             g_att[layer], g_ssm[layer], w_out[layer])
        x = layer_norm(ALPHA * x + gate1 * mix, ln1_g[layer], ln1_b[layer])
        h = x * (1.0 + scale2) + shift2
        ffn = moe_ffn(h, w_router[layer], router_bias[layer], w_e_gate[layer], w_e_up[layer],
                      w_e_down[layer], w_s_gate[layer], w_s_up[layer], w_s_down[layer])
        x = layer_norm(ALPHA * x + gate2 * ffn, ln2_g[layer], ln2_b[layer])
    return x
```

```python
import math
import numpy as np
import concourse.bass as bass
import concourse.mybir as mybir
from concourse.bass_utils import run_bass_kernel_spmd

F32 = mybir.dt.float32
BF16 = mybir.dt.bfloat16
U32 = mybir.dt.uint32
AF = mybir.ActivationFunctionType
ALU = mybir.AluOpType

D = 1024
SEQ = 8192
OWN = 4096
NCORES = 8
NEXP = 256
CAP = 256
CSTR = CAP
BR = 256
NBLK = 384
NROWS = NBLK * BR
ALPHA = 2.0 ** 0.25
EPS = 1e-5
PI = math.pi
DILS = (1, 4, 16)
NEGM = -30000.0


class Sched:
    def __init__(self, nc, n_lanes=8):
        self.nc = nc
        self.eng = {"pe": nc.tensor, "dve": nc.vector, "act": nc.scalar,
                    "pool": nc.gpsimd, "sp": nc.sync}
        self.sem = {}
        self.cnt = {}
        for e in ("pe", "dve", "act", "pool"):
            self.sem[e] = nc.alloc_semaphore("s_" + e)
            self.cnt[e] = 0
        self.lanes = {}
        for q in ("sp", "pool"):
            self.lanes[q] = []
            for i in range(n_lanes):
                k = "l_%s%d" % (q, i)
                self.sem[k] = nc.alloc_semaphore(k)
                self.cnt[k] = 0
                self.lanes[q].append(k)
        self.lane_rr = {q: 0 for q in self.lanes}
        self.seen = {e: {} for e in self.eng}
        self.last_w = {}
        self.readers = {}
        self.nops = 0

    def _deps(self, reads, writes):
        deps = {}

        def add(t):
            if t is not None and deps.get(t[0], 0) < t[1]:
                deps[t[0]] = t[1]
        for r in reads:
            add(self.last_w.get(r))
        for w in writes:
            add(self.last_w.get(w))
            for t in self.readers.get(w, ()):
                add(t)
        return deps

    def _wait(self, e, deps):
        seen = self.seen[e]
        for k, v in deps.items():
            if e == "pe" and k == "pe":
                continue
            if seen.get(k, 0) >= v:
                continue
            self.eng[e].wait_ge(self.sem[k], v)
            seen[k] = v

    def _record(self, ticket, reads, writes):
        for r in reads:
            lst = self.readers.setdefault(r, [])
            lst.append(ticket)
            if len(lst) > 64:
                best = {}
                for k, v in lst:
                    if best.get(k, 0) < v:
                        best[k] = v
                self.readers[r] = list(best.items())
        for w in writes:
            self.last_w[w] = ticket
            self.readers[w] = []

    def op(self, e, fn, reads=(), writes=(), sig=True):
        self._wait(e, self._deps(reads, writes))
        inst = fn(self.eng[e])
        if sig:
            self.cnt[e] += 1
            inst.then_inc(self.sem[e], 1)
            ticket = (e, self.cnt[e])
        else:
            ticket = (e, self.cnt[e] + 1)
        self._record(ticket, reads, writes)
        self.nops += 1
        return inst

    def dma(self, q, fn, reads=(), writes=()):
        lane = self.lanes[q][self.lane_rr[q] % len(self.lanes[q])]
        self.lane_rr[q] += 1
        deps = self._deps(reads, writes)
        if self.cnt[lane] > 0:
            deps[lane] = max(deps.get(lane, 0), 16 * self.cnt[lane])
        self._wait(q, deps)
        inst = fn(self.eng[q])
        self.cnt[lane] += 1
        inst.then_inc(self.sem[lane], 16)
        self._record((lane, 16 * self.cnt[lane]), reads, writes)
        self.nops += 1
        return inst

    def barrier(self):
        final = {}
        for k, c in self.cnt.items():
            if c > 0:
                final[k] = c * (16 if k.startswith("l_") else 1)
        for e in self.eng:
            self._wait(e, dict(final))
        self.last_w = {}
        self.readers = {}


def _t5_bucket(dist):
    exact = 16
    d = np.maximum(dist, 1).astype(np.float32)
    large = exact + (np.log(d / np.float32(exact)) / np.float32(math.log(2048 / exact))
                     * np.float32(32 - exact)).astype(np.int32)
    return np.where(dist < exact, dist, np.minimum(large, 31)).astype(np.int64)


def _bias_layout(rel_bias):
    i = np.arange(128)[:, None]
    j = np.arange(128)[None, :]
    tb = np.zeros((128, 3, 8, 2, 128), np.float32)
    mk = np.zeros((128, 2, 128), np.float32)
    for kt in range(2):
        rel = (j - i) if kt == 1 else (j + 128 - i)
        ok = (rel >= 0) & (rel <= 128)
        mk[:, kt, :] = np.where(ok, 0.0, NEGM)
        for pi, d in enumerate(DILS):
            bk = _t5_bucket(np.maximum(rel, 0) * d)
            for h in range(8):
                tb[:, pi, h, kt, :] = np.where(ok, rel_bias[bk, h], 0.0)
    return tb.reshape(128, 48, 128), mk


def _ssm_layout(a):
    return np.ascontiguousarray(a.reshape(16, 2, 64).transpose(1, 2, 0).reshape(128, 16))


def host_prep(inp):
    f = lambda a: np.ascontiguousarray(a, dtype=np.float32)
    sh = {}
    sh["w_ada"] = f(inp["w_ada"][0])
    sh["b_ada"] = f(inp["b_ada"][0]).reshape(1, 6 * D)
    sh["b_col"] = f(inp["b_ada"][0][:2048].reshape(16, 128).T)
    sh["w_in"] = f(inp["w_in"][0])
    sh["a_re"] = _ssm_layout(f(inp["ssm_a_re"][0]))
    sh["a_im"] = _ssm_layout(f(inp["ssm_a_im"][0]))
    sh["ldt"] = _ssm_layout(np.repeat(f(inp["ssm_log_dt"][0])[:, None], 64, axis=1))
    bl = lambda b: np.ascontiguousarray(b.reshape(16, 2, 64, 16).transpose(1, 2, 0, 3).reshape(128, 256))
    sh["b_re"] = bl(f(inp["ssm_b_re"][0]))
    sh["b_im"] = bl(f(inp["ssm_b_im"][0]))
    cl = lambda c: np.ascontiguousarray(c.reshape(16, 2, 16, 64).transpose(1, 3, 0, 2).reshape(128, 256))
    sh["c_re"] = cl(f(inp["ssm_c_re"][0]))
    sh["c_im"] = cl(f(inp["ssm_c_im"][0]))
    sh["d_l"] = f(inp["ssm_d"][0].reshape(4, 128).T)
    sh["w_glu"] = f(inp["w_glu"][0])
    sh["bglu_l"] = f(inp["b_glu"][0].reshape(4, 128).T)
    sh["gatt_l"] = f(inp["g_att"][0].reshape(4, 128).T)
    sh["gssm_l"] = f(inp["g_ssm"][0].reshape(4, 128).T)
    sh["w_out"] = f(inp["w_out"][0])
    sh["ln1"] = f(np.stack([inp["ln1_g"][0], inp["ln1_b"][0]]))
    sh["ln2"] = f(np.stack([inp["ln2_g"][0], inp["ln2_b"][0]]))
    sh["w_router"] = f(inp["w_router"][0])
    sh["rbias"] = f(inp["router_bias"][0]).reshape(1, NEXP)
    sh["w_eg"] = f(inp["w_e_gate"][0])
    sh["w_eu"] = f(inp["w_e_up"][0])
    sh["w_ed"] = f(inp["w_e_down"][0])
    sh["w_sg"] = f(inp["w_s_gate"][0])
    sh["w_su"] = f(inp["w_s_up"][0])
    sh["w_sd"] = f(inp["w_s_down"][0])
    tb, mk = _bias_layout(f(inp["rel_bias"]))
    sh["tb"] = tb
    sh["mk"] = mk
    sh["ident"] = np.eye(128, dtype=np.float32)
    sh["ltri"] = np.triu(np.ones((128, 128), np.float32), 1)
    sh["mask2"] = np.stack([(np.arange(128) < 64), (np.arange(128) >= 64)], 1).astype(np.float32)
    sh["jidx"] = np.tile(np.arange(512, dtype=np.float32)[None, :], (128, 1))
    sh["ebase"] = np.tile((np.arange(NEXP, dtype=np.float32) * CSTR)[None, :], (128, 1))
    sh["pidx"] = np.arange(128, dtype=np.float32).reshape(128, 1)
    sh["bidx"] = np.tile(np.arange(NBLK, dtype=np.float32)[None, :], (128, 1))
    sh["eidx1"] = np.tile((np.arange(NEXP, dtype=np.float32) + 1.0)[None, :], (128, 1))
    x = f(inp["x"])
    c = f(inp["c"])
    maps = []
    for ci in range(NCORES):
        b, half = ci // 2, ci % 2
        m = dict(sh)
        xs = np.zeros((SEQ, D), np.float32)
        if half == 1:
            xs[:] = x[b]
        else:
            xs[OWN:] = x[b, :OWN]
        m["xs"] = xs
        m["ccol"] = np.ascontiguousarray(c[b].reshape(8, 128).T)
        m["flag"] = np.full((128, 1), float(half), np.float32)
        maps.append(m)
    return maps


def build(shapes, stage=99, dbg=(), lim=16, alim=4, glim=8, blim=NBLK):
    nc = bass.Bass("TRN2", target_bir_lowering=False)
    S = Sched(nc)
    class _Lazy(dict):
        def __missing__(self, k):
            self[k] = nc.dram_tensor(k, list(shapes[k]), F32, kind="ExternalInput").ap()
            return self[k]
    I = _Lazy()
    out = nc.dram_tensor("out", [OWN, D], F32, kind="ExternalOutput").ap()
    dbg_out = {}

    def dbg_dram(name, shape, dt=F32):
        t = nc.dram_tensor("dbg_" + name, list(shape), dt, kind="ExternalOutput").ap()
        dbg_out[name] = t
        return t

    _scr = {"UT": ([512, SEQ], BF16), "KT": ([512, 6144], BF16), "QT": ([512, OWN], BF16),
            "VS": ([6144, 512], BF16), "ADA": ([4, D], F32), "X1": ([OWN, D], F32),
            "SHs": ([OWN, D], F32), "H2": ([OWN, D], BF16), "XSs": ([NROWS, D], BF16), "YSs": ([NROWS, D], BF16)}

    class _LazyScr(dict):
        def __missing__(self, k):
            self[k] = nc.dram_tensor(k, _scr[k][0], _scr[k][1]).ap()
            return self[k]
    R = _LazyScr()

    V = lambda fn, r=(), w=(): S.op("dve", fn, r, w)
    A = lambda fn, r=(), w=(): S.op("act", fn, r, w)
    G = lambda fn, r=(), w=(): S.op("pool", fn, r, w)
    PE = lambda fn, r=(), w=(), sig=True: S.op("pe", fn, r, w, sig)
    DMA = lambda fn, r=(), w=(), q="sp": S.dma(q, fn, r, w)

    from contextlib import ExitStack
    with ExitStack() as top:
        sb = lambda name, shape, dt=F32, ctx=top: ctx.enter_context(nc.sbuf_tensor("s_" + name, list(shape), dt))
        ps = lambda name, shape, dt=F32, ctx=top: ctx.enter_context(nc.psum_tensor("p_" + name, list(shape), dt))

        ident = sb("ident", [128, 128])
        identb = sb("identb", [128, 128], BF16)
        onesb = sb("onesb", [128, 128], BF16)
        ltrib = sb("ltrib", [128, 128], BF16)
        flag = sb("flag", [128, 1])
        s1p = sb("s1p", [128, 8])
        sh1 = sb("sh1", [128, 8])
        DEST = sb("DEST", [128, 32 * 8], U32)
        GATE = sb("GATE", [128, 32 * 8])
        POS = sb("POS", [128, 32 * 8])
        V8S = sb("V8S", [128, 32 * 8])
        IDXW = sb("IDXW", [128, NBLK], U32)
        ssmn = sb("ssmn", [128, 4, OWN], BF16)

        DMA(lambda e: e.dma_start(out=ident[:], in_=I["ident"]), w=["ident"])
        DMA(lambda e: e.dma_start(out=flag[:], in_=I["flag"]), w=["flag"])
        DMA(lambda e: e.dma_start(out=identb[:], in_=I["ident"]), w=["identb"], q="pool")
        DMA(lambda e: e.dma_start(out=ltrib[:], in_=I["ltri"]), w=["ltrib"], q="pool")
        V(lambda e: e.memset(onesb[:], 1.0), w=["onesb"])

        with ExitStack() as ph:
            ccol = sb("ccol", [128, 8], ctx=ph)
            sc = sb("sc", [128, 8], ctx=ph)
            scb = sb("scb", [128, 8, 128], ctx=ph)
            bcol = sb("bcol", [128, 16], ctx=ph)
            bbc = sb("bbc", [128, 4096], ctx=ph)
            wt = [sb("wt%d" % i, [128, 8, 512], ctx=ph) for i in range(2)]
            adab = sb("adab", [128, 512], ctx=ph)
            pcol = ps("pcol", [128, 16], ctx=ph)
            prow = [ps("prow%d" % i, [128, 512], ctx=ph) for i in range(2)]
            DMA(lambda e: e.dma_start(out=ccol[:], in_=I["ccol"]), w=["ccol"])
            DMA(lambda e: e.dma_start(out=bcol[:], in_=I["b_col"]), w=["bcol"])
            DMA(lambda e: e.dma_start(out=bbc[:], in_=I["b_ada"][0:1, 2048:6144].to_broadcast([128, 4096])), w=["bbc"])
            A(lambda e: e.activation(out=sc[:], in_=ccol[:], func=AF.Silu), r=["ccol"], w=["sc"])
            V(lambda e: e.tensor_copy(out=scb[:], in_=sc[:].unsqueeze(2).to_broadcast([128, 8, 128])), r=["sc"], w=["scb"])
            wv = I["w_ada"].rearrange("(kc p) n -> p kc n", p=128)
            for ct in range(12):
                w_ = wt[ct % 2]
                wn = "wt%d" % (ct % 2)
                DMA(lambda e: e.dma_start(out=w_[:], in_=wv[:, :, ct * 512:(ct + 1) * 512]), w=[wn])
                if ct < 4:
                    for j in range(4):
                        col = ct * 4 + j
                        for kc in range(8):
                            PE(lambda e: e.matmul(pcol[:, col:col + 1], lhsT=w_[:, kc, j * 128:(j + 1) * 128],
                                                  rhs=sc[:, kc:kc + 1], start=(kc == 0), stop=(kc == 7)),
                               r=[wn, "sc"], w=["pcol"], sig=(kc == 7))
                else:
                    pr = prow[ct % 2]
                    pn = "prow%d" % (ct % 2)
                    for kc in range(8):
                        PE(lambda e: e.matmul(pr[:], lhsT=scb[:, kc, :], rhs=w_[:, kc, :], start=(kc == 0), stop=(kc == 7)),
                           r=[wn, "scb"], w=[pn], sig=(kc == 7))
                    c0 = (ct - 4) * 512
                    V(lambda e: e.tensor_tensor(out=adab[:], in0=pr[:], in1=bbc[:, c0:c0 + 512], op=ALU.add),
                      r=[pn, "bbc"], w=["adab"])
                    row = (ct - 4) // 2
                    if row == 2:
                        V(lambda e: e.tensor_scalar_add(out=adab[:], in0=adab[:], scalar1=1.0), r=["adab"], w=["adab"])
                    hc = ((ct - 4) % 2) * 512
                    DMA(lambda e: e.dma_start(out=R["ADA"][row:row + 1, hc:hc + 512], in_=adab[0:1, :]), r=["adab"], w=["ADA"])
            V(lambda e: e.tensor_tensor(out=sh1[:], in0=pcol[:, 0:8], in1=bcol[:, 0:8], op=ALU.add), r=["pcol", "bcol"], w=["sh1"])
            V(lambda e: e.scalar_tensor_tensor(out=s1p[:], in0=pcol[:, 8:16], scalar=1.0, in1=bcol[:, 8:16],
                                               op0=ALU.add, op1=ALU.add), r=["pcol", "bcol"], w=["s1p"])
            if "ada" in dbg:
                d1 = dbg_dram("ada_col", [128, 16])
                DMA(lambda e: e.dma_start(out=d1[:, 0:8], in_=sh1[:]), r=["sh1"], w=["dbg"])
                DMA(lambda e: e.dma_start(out=d1[:, 8:16], in_=s1p[:]), r=["s1p"], w=["dbg"])
                d2 = dbg_dram("ada_row", [4, D])
                t4 = sb("t4", [4, D], ctx=ph)
                DMA(lambda e: e.dma_start(out=t4[:], in_=R["ADA"]), r=["ADA"], w=["t4"])
                DMA(lambda e: e.dma_start(out=d2, in_=t4[:]), r=["t4"], w=["dbg2"])
            S.barrier()
        if stage == 0:
            return finish(nc, S, out, dbg_out, I)

        with ExitStack() as ph:
            winb = sb("winb", [128, 8, 2048], BF16, ctx=ph)
            xt = [sb("xt%d" % i, [128, D], ctx=ph) for i in range(3)]
            hT = [sb("hT%d" % i, [128, 8, 512], BF16, ctx=ph) for i in range(2)]
            ev = [sb("ev%d" % i, [128, 4, 512], BF16, ctx=ph) for i in range(4)]
            pT = [ps("pT%d" % i, [128, 1024], ctx=ph) for i in range(2)]
            pp = [ps("pp%d" % i, [128, 512], ctx=ph) for i in range(4)]
            wiv = I["w_in"].rearrange("(kc p) n -> p kc n", p=128)
            for kc in range(8):
                DMA(lambda e: e.dma_start(out=winb[:, kc, :], in_=wiv[:, kc, :]), w=["winb"], q="pool")
            xi = 0
            pi_ = 0
            def stA(st):
                nonlocal xi
                h_ = hT[st % 2]
                hn = "hT%d" % (st % 2)
                for sub in range(4):
                    ti = st * 4 + sub
                    x_ = xt[xi % 3]
                    xn = "xt%d" % (xi % 3)
                    p_ = pT[xi % 2]
                    pn = "pT%d" % (xi % 2)
                    xi += 1
                    DMA(lambda e: e.dma_start(out=x_[:], in_=I["xs"][ti * 128:(ti + 1) * 128, :]), w=[xn])
                    for kc in range(8):
                        PE(lambda e: e.transpose(p_[:, kc * 128:(kc + 1) * 128], x_[:, kc * 128:(kc + 1) * 128], ident[:]),
                           r=[xn, "ident"], w=[pn + "_b%d" % (kc // 4)])
                    for kc in range(8):
                        fn = lambda e: e.activation(out=h_[:, kc, sub * 128:(sub + 1) * 128], in_=p_[:, kc * 128:(kc + 1) * 128],
                                                    func=AF.Identity, scale=s1p[:, kc:kc + 1], bias=sh1[:, kc:kc + 1])
                        A(fn, r=[pn + "_b%d" % (kc // 4), "s1p", "sh1"], w=[hn + "_%d" % sub])
            def stB(st):
                nonlocal pi_
                h_ = hT[st % 2]
                hn = "hT%d" % (st % 2)
                hres = [hn + "_%d" % s_ for s_ in range(4)]
                jobs = [("u", 1536, R["UT"], st * 512, 1.0)]
                if st >= 4:
                    jobs.append(("k", 512, R["KT"], (st - 4) * 512, 1.0))
                if st >= 8:
                    jobs.append(("q", 0, R["QT"], (st - 8) * 512, 0.125))
                for ji, (nm, c0, dst, t0, scl) in enumerate(jobs):
                    e_ = ev[ji]
                    en = "ev%d" % ji
                    for oc in range(4):
                        pq = pp[pi_ % 4]
                        pqn = "pp%d" % (pi_ % 4)
                        pi_ += 1
                        for kc in range(8):
                            PE(lambda e: e.matmul(pq[:], lhsT=winb[:, kc, c0 + oc * 128:c0 + (oc + 1) * 128], rhs=h_[:, kc, :],
                                                  start=(kc == 0), stop=(kc == 7)), r=["winb"] + hres, w=[pqn], sig=(kc == 7))
                        if oc % 2 == 0:
                            A(lambda e: e.activation(out=e_[:, oc, :], in_=pq[:], func=AF.Copy, scale=scl), r=[pqn], w=[en])
                        else:
                            V(lambda e: e.tensor_scalar(out=e_[:, oc, :], in0=pq[:], scalar1=scl, scalar2=None, op0=ALU.mult),
                              r=[pqn], w=[en])
                    DMA(lambda e: e.dma_start(out=dst.rearrange("(oc p) t -> p oc t", p=128)[:, :, t0:t0 + 512], in_=e_[:]),
                        r=[en], w=[nm + "T"], q="pool")
                if st >= 4:
                    e_ = ev[3]
                    for sub in range(4):
                        pq = pp[pi_ % 4]
                        pqn = "pp%d" % (pi_ % 4)
                        pi_ += 1
                        for kc in range(8):
                            PE(lambda e: e.matmul(pq[:], lhsT=h_[:, kc, sub * 128:(sub + 1) * 128], rhs=winb[:, kc, 1024:1536],
                                                  start=(kc == 0), stop=(kc == 7)), r=["winb"] + hres, w=[pqn], sig=(kc == 7))
                        if sub % 2 == 0:
                            A(lambda e: e.activation(out=e_[:, sub, :], in_=pq[:], func=AF.Copy), r=[pqn], w=["ev3"])
                        else:
                            V(lambda e: e.tensor_copy(out=e_[:, sub, :], in_=pq[:]), r=[pqn], w=["ev3"])
                    r0 = (st - 4) * 512
                    DMA(lambda e: e.dma_start(out=R["VS"][r0:r0 + 512, :].rearrange("(s p) f -> p s f", p=128), in_=e_[:]),
                        r=["ev3"], w=["VS"], q="pool")
            n_st = 16 if lim >= 16 else lim
            stA(0)
            for st in range(n_st):
                if st + 1 < n_st:
                    stA(st + 1)
                stB(st)
            if "proj" in dbg:
                for nm, src, shp, rn in (("UT", R["UT"], [512, SEQ], "uT"), ("KT", R["KT"], [512, 6144], "kT"),
                                         ("QT", R["QT"], [512, OWN], "qT"), ("VS", R["VS"], [6144, 512], "VS")):
                    dd = dbg_dram(nm, shp, BF16)
                    DMA(lambda e: e.dma_start(out=dd, in_=src), r=[rn], w=["dbgo" + nm])
            S.barrier()
        if stage == 1:
            return finish(nc, S, out, dbg_out, I)


        with ExitStack() as ph:
            P_ = lambda name, shape, dt=F32: sb(name, shape, dt, ctx=ph)
            are, aim, ldt = P_("are", [128, 16]), P_("aim", [128, 16]), P_("ldt", [128, 16])
            bre, bim = P_("bre", [128, 16, 16]), P_("bim", [128, 16, 16])
            cre, cim = P_("cre", [128, 16, 16]), P_("cim", [128, 16, 16])
            mask2, jidx = P_("mask2", [128, 2]), P_("jidx", [128, 512])
            d_l, bglu, gssm = P_("d_l", [128, 4]), P_("bglu", [128, 4]), P_("gssm", [128, 4])
            wglub = P_("wglub", [128, 4, 512], BF16)
            for t_, k_ in ((are, "a_re"), (aim, "a_im"), (ldt, "ldt"), (mask2, "mask2"), (jidx, "jidx"),
                           (d_l, "d_l"), (bglu, "bglu_l"), (gssm, "gssm_l")):
                DMA(lambda e: e.dma_start(out=t_[:], in_=I[k_]), w=[k_])
            for t_, k_ in ((bre, "b_re"), (bim, "b_im"), (cre, "c_re"), (cim, "c_im")):
                DMA(lambda e: e.dma_start(out=t_[:].rearrange("p q c -> p (q c)"), in_=I[k_]), w=[k_])
            DMA(lambda e: e.dma_start(out=wglub[:], in_=I["w_glu"].rearrange("(cc p) n -> p cc n", p=128)), w=["wglub"], q="pool")
            sm = {n_: P_(n_, [128, 16]) for n_ in ("dt", "xre", "th", "rho", "m1", "sn", "cs", "abr", "abi", "nr",
                                                   "den", "t1", "t2", "cr", "ci", "c512", "s512", "glr", "gli",
                                                   "car", "cai", "u1", "u2")}
            tt = lambda o, a, b, op, r, w: V(lambda e: e.tensor_tensor(out=o, in0=a, in1=b, op=op), r=r, w=w)

            I32 = mybir.dt.int32
            C1, C2 = 6.28125, 2 * PI - 6.28125
            rk_i = P_("rk_i", [128, 512], I32)
            wk = [{n_: P_("%s%d" % (n_, i), [128, 512]) for n_ in ("ta", "tb", "wre", "wim", "gre", "gim")} for i in range(2)]
            rk_f, rk_r, rk_x = wk[1]["ta"], wk[1]["tb"], wk[1]["wre"]

            def sin_rr(out_ap, x_ap, x_res, n, w_res):
                ki, kf, r = rk_i[:, 0:n], rk_f[:, 0:n], rk_r[:, 0:n]
                V(lambda e: e.tensor_scalar(out=ki, in0=x_ap, scalar1=1.0 / (2 * PI), scalar2=None, op0=ALU.mult), r=x_res, w=["rk_i"])
                V(lambda e: e.tensor_copy(out=kf, in_=ki), r=["rk_i"], w=["rk_f"])
                V(lambda e: e.scalar_tensor_tensor(out=r, in0=kf, scalar=-C1, in1=x_ap, op0=ALU.mult, op1=ALU.add), r=["rk_f"] + x_res, w=["rk_r"])
                V(lambda e: e.scalar_tensor_tensor(out=r, in0=kf, scalar=-C2, in1=r, op0=ALU.mult, op1=ALU.add), r=["rk_f", "rk_r"], w=["rk_r"])
                V(lambda e: e.tensor_scalar(out=kf, in0=r, scalar1=PI, scalar2=-2 * PI, op0=ALU.is_gt, op1=ALU.mult), r=["rk_r"], w=["rk_f"])
                V(lambda e: e.tensor_tensor(out=r, in0=r, in1=kf, op=ALU.add), r=["rk_r", "rk_f"], w=["rk_r"])
                V(lambda e: e.tensor_scalar(out=r, in0=r, scalar1=-PI, scalar2=PI, op0=ALU.max, op1=ALU.min), r=["rk_r"], w=["rk_r"])
                A(lambda e: e.activation(out=out_ap, in_=r, func=AF.Sin), r=["rk_r"], w=w_res)

            def sincos(x_ap, x_res, sn_ap, cs_ap, n, sn_res, cs_res):
                V(lambda e: e.tensor_scalar_add(out=rk_x[:, 0:n], in0=x_ap, scalar1=0.5 * PI), r=x_res, w=["rk_x"])
                sin_rr(cs_ap, rk_x[:, 0:n], ["rk_x"], n, cs_res)
                sin_rr(sn_ap, x_ap, x_res, n, sn_res)

            A(lambda e: e.activation(out=sm["dt"][:], in_=ldt[:], func=AF.Exp), r=["ldt"], w=["dt"])
            tt(sm["xre"][:], sm["dt"][:], are[:], ALU.mult, ["dt", "a_re"], ["xre"])
            tt(sm["th"][:], sm["dt"][:], aim[:], ALU.mult, ["dt", "a_im"], ["th"])
            A(lambda e: e.activation(out=sm["rho"][:], in_=sm["xre"][:], func=AF.Exp), r=["xre"], w=["rho"])
            sincos(sm["th"][:], ["th"], sm["sn"][:], sm["cs"][:], 16, ["sc_sn"], ["sc_cs"])
            tt(sm["abr"][:], sm["rho"][:], sm["cs"][:], ALU.mult, ["rho", "sc_cs"], ["abr"])
            tt(sm["abi"][:], sm["rho"][:], sm["sn"][:], ALU.mult, ["rho", "sc_sn"], ["abi"])
            V(lambda e: e.tensor_scalar_add(out=sm["nr"][:], in0=sm["abr"][:], scalar1=-1.0), r=["abr"], w=["nr"])
            tt(sm["t1"][:], are[:], are[:], ALU.mult, ["a_re"], ["t1"])
            tt(sm["t2"][:], aim[:], aim[:], ALU.mult, ["a_im"], ["t2"])
            tt(sm["den"][:], sm["t1"][:], sm["t2"][:], ALU.add, ["t1", "t2"], ["den"])
            V(lambda e: e.reciprocal(out=sm["den"][:], in_=sm["den"][:]), r=["den"], w=["den"])
            tt(sm["t1"][:], sm["nr"][:], are[:], ALU.mult, ["nr", "a_re", "den"], ["t1"])
            tt(sm["t2"][:], sm["abi"][:], aim[:], ALU.mult, ["abi", "a_im", "den"], ["t2"])
            tt(sm["cr"][:], sm["t1"][:], sm["t2"][:], ALU.add, ["t1", "t2"], ["cr"])
            tt(sm["cr"][:], sm["cr"][:], sm["den"][:], ALU.mult, ["cr", "den"], ["cr"])
            tt(sm["t1"][:], sm["abi"][:], are[:], ALU.mult, ["abi", "a_re", "cr"], ["t1"])
            tt(sm["t2"][:], sm["nr"][:], aim[:], ALU.mult, ["nr", "a_im", "cr"], ["t2"])
            tt(sm["ci"][:], sm["t1"][:], sm["t2"][:], ALU.subtract, ["t1", "t2"], ["ci"])
            tt(sm["ci"][:], sm["ci"][:], sm["den"][:], ALU.mult, ["ci", "den"], ["ci"])
            bbr, bbi, tq = P_("bbr", [128, 16, 16]), P_("bbi", [128, 16, 16]), P_("tq", [128, 16, 16])
            crb = sm["cr"][:].unsqueeze(2).to_broadcast([128, 16, 16])
            cib = sm["ci"][:].unsqueeze(2).to_broadcast([128, 16, 16])
            tt(bbr[:], bre[:], crb, ALU.mult, ["b_re", "cr"], ["bbr"])
            tt(tq[:], bim[:], cib, ALU.mult, ["b_im", "ci"], ["tq"])
            tt(bbr[:], bbr[:], tq[:], ALU.subtract, ["bbr", "tq"], ["bbr"])
            tt(bbi[:], bim[:], crb, ALU.mult, ["b_im", "cr"], ["bbi"])
            tt(tq[:], bre[:], cib, ALU.mult, ["b_re", "ci", "bbr"], ["tq"])
            tt(bbi[:], bbi[:], tq[:], ALU.add, ["bbi", "tq"], ["bbi"])
            E = P_("E", [128, 16, 2, 16])
            TB = [P_("TB%d" % i, [128, 4, 128], BF16) for i in range(2)]
            CM = [P_("CM%d" % i, [128, 16, 2, 16], BF16) for i in range(2)]
            bu = [ps("bu%d" % i, [128, 512], ctx=ph) for i in range(4)]
            for ri, (src, sn_) in enumerate(((bbr, "bbr"), (bbi, "bbi"))):
                for g_ in range(2):
                    V(lambda e: e.tensor_scalar(out=E[:, :, g_, :], in0=src[:], scalar1=mask2[:, g_:g_ + 1], scalar2=None, op0=ALU.mult),
                      r=[sn_, "mask2"], w=["E"])
                for cc in range(4):
                    PE(lambda e: e.transpose(bu[ri][:, cc * 128:(cc + 1) * 128],
                                             E[:, 4 * cc:4 * cc + 4, :, :].rearrange("p q g c -> p (q g c)"), ident[:]),
                       r=["E", "ident"], w=["bu%d" % ri])
                V(lambda e: e.tensor_copy(out=TB[ri][:].rearrange("p c s -> p (c s)"), in_=bu[ri][:]), r=["bu%d" % ri], w=["TB%d" % ri])
            for g_ in range(2):
                V(lambda e: e.tensor_scalar(out=CM[0][:, :, g_, :], in0=cre[:], scalar1=mask2[:, g_:g_ + 1], scalar2=None, op0=ALU.mult),
                  r=["c_re", "mask2"], w=["CM0"])
                V(lambda e: e.tensor_scalar(out=CM[1][:, :, g_, :], in0=cim[:], scalar1=mask2[:, g_:g_ + 1], scalar2=-1.0, op0=ALU.mult, op1=ALU.mult),
                  r=["c_im", "mask2"], w=["CM1"])
            COST, SINT = P_("COST", [128, 16, 512]), P_("SINT", [128, 16, 512])
            for q in range(16):
                V(lambda e: e.tensor_scalar(out=SINT[:, q, :], in0=jidx[:], scalar1=sm["th"][:, q:q + 1], scalar2=None, op0=ALU.mult),
                  r=["th", "jidx"], w=["SINT"])
                sincos(SINT[:, q, :], ["SINT"], SINT[:, q, :], COST[:, q, :], 512, ["SINT"], ["COST"])
            V(lambda e: e.tensor_scalar(out=sm["u1"][:], in0=sm["th"][:], scalar1=512.0, scalar2=None, op0=ALU.mult), r=["th"], w=["u1"])
            sincos(sm["u1"][:], ["u1"], sm["s512"][:], sm["c512"][:], 16, ["s512"], ["c512"])
            V(lambda e: e.memset(sm["car"][:], 0.0), w=["car"])
            V(lambda e: e.memset(sm["cai"][:], 0.0), w=["cai"])

            uT = [P_("uT%d" % i, [128, 4, 512], BF16) for i in range(2)]
            S.barrier()
            wo = [{n_: P_("%s%d" % (n_, i), [128, 512]) for n_ in ("oa", "ob")} for i in range(2)]
            hb = [{n_: P_("%s%d" % (n_, i), [128, 512], BF16) for n_ in ("hre", "him")} for i in range(2)]
            ypre, sq_, inn = P_("ypre", [128, 512]), P_("sq_", [128, 512]), P_("inn", [128, 512])
            yg, ygb = P_("yg", [128, 4, 512]), P_("ygb", [128, 4, 512], BF16)
            s_, sqb = P_("s_", [128, 4, 512]), P_("sqb", [128, 4, 512], BF16)
            rs = P_("rs", [128, 512])
            yps = [ps("yps%d" % i, [128, 512], ctx=ph) for i in range(2)]
            zps = [ps("zps%d" % i, [128, 512], ctx=ph) for i in range(2)]
            UTv = R["UT"].rearrange("(cc p) t -> p cc t", p=128)
            zt = P_("zt", [128, 2048], BF16)
            V(lambda e: e.memset(zt[:], 0.0), w=["zt"])
            xsz = R["XSs"].rearrange("(n p r) f -> n p (r f)", p=128, r=2)
            nz_st = NROWS // 256 // 16
            for st in range(16 if lim >= 16 else min(lim, 16)):
                u_ = uT[st % 2]
                un = "uT%d" % (st % 2)
                if st == 0:
                    DMA(lambda e: e.dma_start(out=u_[:], in_=UTv[:, :, 0:512]), w=[un])
                if st + 1 < 16:
                    DMA(lambda e: e.dma_start(out=uT[(st + 1) % 2][:], in_=UTv[:, :, (st + 1) * 512:(st + 2) * 512]), w=["uT%d" % ((st + 1) % 2)])
                own = st >= 8
                pend = []
                for zi in range(nz_st * st, nz_st * (st + 1)):
                    DMA(lambda e: e.dma_start(out=xsz[zi], in_=zt[:]), r=["zt"], w=["XSs"])
                def fin_chunk(cc):
                    yp = yps[cc % 2]
                    ypn = "yps%d" % (cc % 2)
                    V(lambda e: e.scalar_tensor_tensor(out=ypre[:], in0=u_[:, cc, :], scalar=d_l[:, cc:cc + 1], in1=yp[:],
                                                       op0=ALU.mult, op1=ALU.add), r=[un, "d_l", ypn], w=["ypre"])
                    A(lambda e: e.activation(out=sq_[:], in_=ypre[:], func=AF.Square), r=["ypre"], w=["sq_"])
                    V(lambda e: e.tensor_scalar(out=inn[:], in0=sq_[:], scalar1=0.044715, scalar2=1.0, op0=ALU.mult, op1=ALU.add), r=["sq_"], w=["inn"])
                    tt(inn[:], inn[:], ypre[:], ALU.mult, ["inn", "ypre"], ["inn"])
                    A(lambda e: e.activation(out=sq_[:], in_=inn[:], func=AF.Sigmoid, scale=2.0 * math.sqrt(2.0 / PI)), r=["inn"], w=["sq_"])
                    tt(yg[:, cc, :], ypre[:], sq_[:], ALU.mult, ["ypre", "sq_"], ["yg%d" % cc])
                    A(lambda e: e.activation(out=ygb[:, cc, :], in_=yg[:, cc, :], func=AF.Copy), r=["yg%d" % cc], w=["ygb%d" % cc])

                def emit_bu(q_):
                    cc_, ql_, k_ = q_ // 4, q_ % 4, q_ % 2
                    PE(lambda e: e.matmul(bu[2 * k_][:], lhsT=TB[0][32 * ql_:32 * ql_ + 32, cc_, :], rhs=u_[32 * ql_:32 * ql_ + 32, cc_, :],
                                          start=True, stop=True, tile_position=(32 * ql_, 0)), r=["TB0", un], w=["bu%d" % (2 * k_)])
                    PE(lambda e: e.matmul(bu[2 * k_ + 1][:], lhsT=TB[1][32 * ql_:32 * ql_ + 32, cc_, :], rhs=u_[32 * ql_:32 * ql_ + 32, cc_, :],
                                          start=True, stop=True, tile_position=(32 * ql_, 0)), r=["TB1", un], w=["bu%d" % (2 * k_ + 1)])

                for q in range(16):
                    cc, ql = q // 4, q % 4
                    k = q % 2
                    W = wk[k]
                    wn = lambda n_: "%s%d" % (n_, k)
                    pr, pi2 = bu[2 * k], bu[2 * k + 1]
                    prn, pin = "bu%d" % (2 * k), "bu%d" % (2 * k + 1)
                    if q == 0:
                        emit_bu(0)
                    C_, S_ = COST[:, q, :], SINT[:, q, :]
                    tt(W["ta"][:], pr[:], C_, ALU.mult, [prn, "COST"], [wn("ta")])
                    tt(W["tb"][:], pi2[:], S_, ALU.mult, [pin, "SINT"], [wn("tb")])
                    tt(W["wre"][:], W["ta"][:], W["tb"][:], ALU.add, [wn("ta"), wn("tb")], [wn("wre")])
                    if own:
                        tt(W["ta"][:], pi2[:], C_, ALU.mult, [pin, "COST", wn("wre")], [wn("ta")])
                        tt(W["tb"][:], pr[:], S_, ALU.mult, [prn, "SINT", wn("wre")], [wn("tb")])
                        tt(W["wim"][:], W["ta"][:], W["tb"][:], ALU.subtract, [wn("ta"), wn("tb")], [wn("wim")])
                    else:
                        Op = wo[k]
                        A(lambda e: e.activation(out=Op["oa"][:], in_=pr[:], func=AF.Copy), r=[prn], w=["oa%d" % k, prn])
                        A(lambda e: e.activation(out=Op["ob"][:], in_=pi2[:], func=AF.Copy), r=[pin], w=["ob%d" % k, pin])
                        G(lambda e: e.tensor_tensor(out=W["wim"][:], in0=Op["ob"][:], in1=C_, op=ALU.mult), r=["ob%d" % k, "COST"], w=[wn("wim")])
                        G(lambda e: e.tensor_tensor(out=Op["oa"][:], in0=Op["oa"][:], in1=S_, op=ALU.mult), r=["oa%d" % k, "SINT"], w=["oa%d" % k])
                        G(lambda e: e.tensor_tensor(out=W["wim"][:], in0=W["wim"][:], in1=Op["oa"][:], op=ALU.subtract), r=[wn("wim"), "oa%d" % k], w=[wn("wim")])
                    rb = sm["rho"][:, q:q + 1].to_broadcast([128, 512])
                    V(lambda e: e.tensor_tensor_scan(out=W["gre"][:], data0=rb, data1=W["wre"][:], initial=sm["car"][:, q:q + 1],
                                                     op0=ALU.mult, op1=ALU.add), r=["rho", wn("wre"), "car"], w=[wn("gre")])
                    V(lambda e: e.tensor_tensor_scan(out=W["gim"][:], data0=rb, data1=W["wim"][:], initial=sm["cai"][:, q:q + 1],
                                                     op0=ALU.mult, op1=ALU.add), r=["rho", wn("wim"), "cai"], w=[wn("gim")])
                    A(lambda e: e.activation(out=sm["glr"][:, q:q + 1], in_=W["gre"][:, 511:512], func=AF.Copy), r=[wn("gre")], w=["glr"])
                    A(lambda e: e.activation(out=sm["gli"][:, q:q + 1], in_=W["gim"][:, 511:512], func=AF.Copy), r=[wn("gim")], w=["gli"])
                    if q + 1 < 16:
                        emit_bu(q + 1)
                    if own:
                        O, H = wo[k], hb[k]
                        on = lambda n_: "%s%d" % (n_, k)
                        gt = lambda o, a, b, op, r, w: G(lambda e: e.tensor_tensor(out=o, in0=a, in1=b, op=op), r=r, w=w)
                        tt(O["oa"][:], W["gre"][:], C_, ALU.mult, [wn("gre"), "COST"], [on("oa")])
                        gt(O["ob"][:], W["gim"][:], S_, ALU.mult, [wn("gim"), "SINT"], [on("ob")])
                        gt(H["hre"][:], O["oa"][:], O["ob"][:], ALU.subtract, [on("oa"), on("ob")], [on("hre")])
                        gt(O["oa"][:], W["gre"][:], S_, ALU.mult, [wn("gre"), "SINT", on("hre")], [on("oa")])
                        gt(O["ob"][:], W["gim"][:], C_, ALU.mult, [wn("gim"), "COST", on("hre")], [on("ob")])
                        gt(H["him"][:], O["oa"][:], O["ob"][:], ALU.add, [on("oa"), on("ob")], [on("him")])
                        yp = yps[cc % 2]
                        ypn = "yps%d" % (cc % 2)
                        PE(lambda e: e.matmul(yp[32 * ql:32 * ql + 32, :], lhsT=CM[0][:, q, :, :].rearrange("p g c -> p (g c)"), rhs=H["hre"][:],
                                              start=True, stop=False, tile_position=(0, 32 * ql)), r=["CM0", on("hre")], w=[ypn], sig=False)
                        PE(lambda e: e.matmul(yp[32 * ql:32 * ql + 32, :], lhsT=CM[1][:, q, :, :].rearrange("p g c -> p (g c)"), rhs=H["him"][:],
                                              start=False, stop=True, tile_position=(0, 32 * ql)), r=["CM1", on("him")], w=[ypn])
                        if ql == 3:
                            pend.append(cc)
                    if ql == 1 and pend:
                        fin_chunk(pend.pop(0))
                tt(sm["t1"][:], sm["glr"][:], sm["c512"][:], ALU.mult, ["glr", "c512"], ["t1"])
                tt(sm["t2"][:], sm["gli"][:], sm["s512"][:], ALU.mult, ["gli", "s512"], ["t2"])
                tt(sm["car"][:], sm["t1"][:], sm["t2"][:], ALU.subtract, ["t1", "t2"], ["car"])
                tt(sm["t1"][:], sm["glr"][:], sm["s512"][:], ALU.mult, ["glr", "s512", "car"], ["t1"])
                tt(sm["t2"][:], sm["gli"][:], sm["c512"][:], ALU.mult, ["gli", "c512", "car"], ["t2"])
                tt(sm["cai"][:], sm["t1"][:], sm["t2"][:], ALU.add, ["t1", "t2"], ["cai"])
                while pend:
                    fin_chunk(pend.pop(0))
                if st == 7:
                    V(lambda e: e.tensor_scalar(out=sm["car"][:], in0=sm["car"][:], scalar1=flag[:, 0:1], scalar2=None, op0=ALU.mult), r=["car", "flag"], w=["car"])
                    V(lambda e: e.tensor_scalar(out=sm["cai"][:], in0=sm["cai"][:], scalar1=flag[:, 0:1], scalar2=None, op0=ALU.mult), r=["cai", "flag"], w=["cai"])
                if own:
                    t0 = (st - 8) * 512
                    ygr = ["ygb%d" % c_ for c_ in range(4)]
                    for oc in range(4):
                        zp = zps[oc % 2]
                        zn = "zps%d" % (oc % 2)
                        for cc in range(4):
                            PE(lambda e: e.matmul(zp[:], lhsT=wglub[:, cc, oc * 128:(oc + 1) * 128], rhs=ygb[:, cc, :], start=(cc == 0), stop=(cc == 3)),
                               r=["wglub"] + ygr, w=[zn], sig=(cc == 3))
                        A(lambda e: e.activation(out=rs[:], in_=zp[:], func=AF.Sigmoid, bias=bglu[:, oc:oc + 1]), r=[zn, "bglu_l"], w=["rs"])
                        tt(s_[:, oc, :], yg[:, oc, :], rs[:], ALU.mult, ["yg%d" % oc, "rs"], ["s_%d" % oc])
                        A(lambda e: e.activation(out=sqb[:, oc, :], in_=s_[:, oc, :], func=AF.Square), r=["s_%d" % oc], w=["sqb%d" % oc])
                    zp = zps[0]
                    for oc in range(4):
                        PE(lambda e: e.matmul(zp[:], lhsT=onesb[:], rhs=sqb[:, oc, :], start=(oc == 0), stop=(oc == 3)),
                           r=["onesb"] + ["sqb%d" % o_ for o_ in range(4)], w=["zps0"], sig=(oc == 3))
                    V(lambda e: e.tensor_scalar(out=rs[:], in0=zp[:], scalar1=1.0 / 512, scalar2=EPS, op0=ALU.mult, op1=ALU.add), r=["zps0"], w=["rs"])
                    A(lambda e: e.activation(out=rs[:], in_=rs[:], func=AF.Sqrt), r=["rs"], w=["rs"])
                    V(lambda e: e.reciprocal(out=rs[:], in_=rs[:]), r=["rs"], w=["rs"])
                    for oc in range(4):
                        V(lambda e: e.scalar_tensor_tensor(out=ssmn[:, oc, t0:t0 + 512], in0=s_[:, oc, :], scalar=gssm[:, oc:oc + 1], in1=rs[:],
                                                           op0=ALU.mult, op1=ALU.mult), r=["s_%d" % oc, "gssm_l", "rs"], w=["ssmn"])
            if "ssm" in dbg:
                dd = dbg_dram("ssmn", [512, OWN], BF16)
                DMA(lambda e: e.dma_start(out=dd.rearrange("(oc p) t -> p oc t", p=128), in_=ssmn[:]), r=["ssmn"], w=["dbgssm"])
            S.barrier()
        if stage == 2:
            return finish(nc, S, out, dbg_out, I)


        ph23 = top.enter_context(ExitStack())
        attT = sb("attT", [128, 4, OWN], BF16, ctx=ph23)
        with ExitStack() as ph:
            P_ = lambda name, shape, dt=F32: sb(name, shape, dt, ctx=ph)
            tbf = P_("tbf", [128, 24, 2, 128])
            mkt = P_("mkt", [128, 2, 128])
            biasb = P_("biasb", [128, 24, 2, 128], BF16)
            prev0b = P_("prev0b", [128, 24, 128], BF16)
            pm = P_("pm", [128, 1])
            ones64 = P_("ones64", [128, 64], BF16)
            qTc = P_("qTc", [128, OWN], BF16)
            kTc = P_("kTc", [128, 6144], BF16)
            Vd = [P_("Vd%d" % i, [128, 48, 128], BF16) for i in range(3)]
            acc = P_("acc", [128, 2, OWN])
            pTs = [P_("pTs%d" % i, [128, 2, 2, 128], BF16) for i in range(2)]
            sp = [ps("sp%d" % i, [128, 2, 2, 128], ctx=ph) for i in range(2)]
            opp = [ps("opp%d" % i, [128, 4, 128], ctx=ph) for i in range(2)]
            DMA(lambda e: e.dma_start(out=tbf[:].rearrange("p a k j -> p (a k) j"), in_=I["tb"]), w=["tbf"])
            DMA(lambda e: e.dma_start(out=mkt[:], in_=I["mk"]), w=["mkt"])
            V(lambda e: e.memset(ones64[:], 1.0), w=["ones64"])
            V(lambda e: e.tensor_scalar(out=pm[:], in0=flag[:], scalar1=-1.0, scalar2=-NEGM, op0=ALU.add, op1=ALU.mult), r=["flag"], w=["pm"])
            for kt in range(2):
                V(lambda e: e.tensor_tensor(out=biasb[:, :, kt, :], in0=tbf[:, :, kt, :], in1=mkt[:, kt, :].unsqueeze(1).to_broadcast([128, 24, 128]),
                                            op=ALU.add), r=["tbf", "mkt"], w=["biasb"])
            V(lambda e: e.tensor_scalar(out=prev0b[:], in0=biasb[:, :, 0, :], scalar1=pm[:, 0:1], scalar2=None, op0=ALU.add), r=["biasb", "pm"], w=["prev0b"])
            for c in range(alim):
                DMA(lambda e: e.dma_start(out=qTc[:], in_=R["QT"][c * 128:(c + 1) * 128, :]), w=["qTc"])
                DMA(lambda e: e.dma_start(out=kTc[:], in_=R["KT"][c * 128:(c + 1) * 128, :]), w=["kTc"])
                for pi, d in enumerate(DILS):
                    vv = R["VS"].rearrange("(m i d) f -> d i m f", i=128, d=d)
                    nt = 48 // d
                    for r in range(d):
                        DMA(lambda e: e.dma_start(out=Vd[pi][:, r * nt:(r + 1) * nt, :], in_=vv[r][:, :, c * 128:(c + 1) * 128]), w=["Vd%d" % pi])
                units = [(pi, d, r, n) for pi, d in enumerate(DILS) for r in range(d) for n in range(32 // d)]

                def stS(ui):
                    pi, d, r, n = units[ui]
                    m_cur = 16 // d + n
                    b = ui % 2
                    sp_, pT_ = sp[b], pTs[b]
                    q0 = r + 128 * n * d
                    qsl = slice(q0, q0 + 127 * d + 1, d)
                    for hh in range(2):
                        hs = slice(64 * hh, 64 * hh + 64)
                        for kt, m in ((0, m_cur - 1), (1, m_cur)):
                            k0 = r + 128 * m * d
                            PE(lambda e: e.matmul(sp_[:, hh, kt, :], lhsT=kTc[hs, k0:k0 + 127 * d + 1:d], rhs=qTc[hs, qsl],
                                                  start=True, stop=False, tile_position=(64 * hh, 0)), r=["kTc", "qTc"], w=["sp%d" % b], sig=False)
                            ph_ = pi * 8 + 2 * c + hh
                            bt = prev0b[:, ph_, :] if (kt == 0 and n == 0) else biasb[:, ph_, kt, :]
                            PE(lambda e: e.matmul(sp_[:, hh, kt, :], lhsT=identb[:], rhs=bt, start=False, stop=True),
                               r=["identb", "biasb", "prev0b"], w=["sp%d" % b], sig=(hh == 1 and kt == 1))
                    A(lambda e: e.activation(out=pT_[:].rearrange("p a b j -> p (a b j)"), in_=sp_[:].rearrange("p a b j -> p (a b j)"), func=AF.Exp),
                      r=["sp%d" % b], w=["pTs%d" % b])

                def stPV(ui):
                    pi, d, r, n = units[ui]
                    nt = 48 // d
                    m_cur = 16 // d + n
                    b = ui % 2
                    op_, pT_ = opp[b], pTs[b]
                    q0 = r + 128 * n * d
                    qsl = slice(q0, q0 + 127 * d + 1, d)
                    for hh in range(2):
                        hs = slice(64 * hh, 64 * hh + 64)
                        for kt, m in ((0, m_cur - 1), (1, m_cur)):
                            PE(lambda e: e.matmul(op_[hs, 0, :], lhsT=Vd[pi][:, r * nt + m, hs], rhs=pT_[:, hh, kt, :],
                                                  start=(kt == 0), stop=(kt == 1), tile_position=(0, 64 * hh)),
                               r=["Vd%d" % pi, "pTs%d" % b], w=["opp%d" % b], sig=False)
                        for kt in range(2):
                            PE(lambda e: e.matmul(op_[hs, 1, :], lhsT=ones64[:], rhs=pT_[:, hh, kt, :],
                                                  start=(kt == 0), stop=(kt == 1), tile_position=(0, 64 * hh)),
                               r=["ones64", "pTs%d" % b], w=["opp%d" % b], sig=(hh == 1 and kt == 1))
                    if pi == 0:
                        V(lambda e: e.tensor_copy(out=acc[:, :, qsl], in_=op_[:, 0:2, :]), r=["opp%d" % b], w=["acc"])
                    else:
                        V(lambda e: e.tensor_tensor(out=acc[:, :, qsl], in0=op_[:, 0:2, :], in1=acc[:, :, qsl], op=ALU.add), r=["opp%d" % b, "acc"], w=["acc"])

                stS(0)
                for ui in range(len(units)):
                    if ui + 1 < len(units):
                        stS(ui + 1)
                    stPV(ui)
                V(lambda e: e.reciprocal(out=acc[:, 1, :], in_=acc[:, 1, :]), r=["acc"], w=["acc"])
                V(lambda e: e.tensor_tensor(out=attT[:, c, :], in0=acc[:, 0, :], in1=acc[:, 1, :], op=ALU.mult), r=["acc"], w=["attT"])
            if "att" in dbg:
                dd = dbg_dram("attT", [512, OWN], BF16)
                DMA(lambda e: e.dma_start(out=dd.rearrange("(oc p) t -> p oc t", p=128)[:, 0:alim, :], in_=attT[:, 0:alim, :]), r=["attT"], w=["dbgatt"])
            S.barrier()
        if stage == 3:
            return finish(nc, S, out, dbg_out, I)


        def bcast_row(dst, src_row, name):
            DMA(lambda e: e.dma_start(out=dst[:], in_=src_row.to_broadcast([128, src_row.shape[1]])), r=["ADA"], w=[name])

        def layer_norm(xr, xres, outt, outres, g_t, b_t, gname, bname, st6, mv, rstd):
            for hf in range(2):
                V(lambda e: e.bn_stats(out=st6[:, hf, :], in_=xr[:, hf * 512:(hf + 1) * 512]), r=[xres], w=["st6"])
            V(lambda e: e.bn_aggr(out=mv[:], in_=st6[:].rearrange("p a b -> p (a b)")), r=["st6"], w=["mv"])
            V(lambda e: e.tensor_scalar_add(out=rstd[:], in0=mv[:, 1:2], scalar1=EPS), r=["mv"], w=["rstd"])
            A(lambda e: e.activation(out=rstd[:], in_=rstd[:], func=AF.Sqrt), r=["rstd"], w=["rstd"])
            V(lambda e: e.reciprocal(out=rstd[:], in_=rstd[:]), r=["rstd"], w=["rstd"])
            V(lambda e: e.tensor_scalar(out=outt[:], in0=xr[:], scalar1=mv[:, 0:1], scalar2=rstd[:, 0:1], op0=ALU.subtract, op1=ALU.mult),
              r=[xres, "mv", "rstd"], w=[outres])
            V(lambda e: e.tensor_tensor(out=outt[:], in0=outt[:], in1=g_t[:], op=ALU.mult), r=[outres, gname], w=[outres])
            V(lambda e: e.tensor_tensor(out=outt[:], in0=outt[:], in1=b_t[:], op=ALU.add), r=[outres, bname], w=[outres])

        with ExitStack() as ph:
            P_ = lambda name, shape, dt=F32: sb(name, shape, dt, ctx=ph)
            woutg = P_("woutg", [128, 8, D], BF16)
            wrb = P_("wrb", [128, 8, NEXP], BF16)
            wsg, wsu = P_("wsg", [128, 8, 256], BF16), P_("wsu", [128, 8, 256], BF16)
            wsd = P_("wsd", [128, 2, D], BF16)
            g1bc, ln1g, ln1b, sc2, sh2 = (P_(n_, [128, D]) for n_ in ("g1bc", "ln1g", "ln1b", "sc2", "sh2"))
            rbbc, ebase, eidx1, cntbc = (P_(n_, [128, NEXP]) for n_ in ("rbbc", "ebase", "eidx1", "cntbc"))
            gatt = P_("gatt", [128, 4])
            wst = [P_("wst0", [128, D])] * 2
            bcast_row(g1bc, R["ADA"][0:1, :], "g1bc")
            bcast_row(sh2, R["ADA"][1:2, :], "sh2")
            bcast_row(sc2, R["ADA"][2:3, :], "sc2")
            bcast_row(ln1g, I["ln1"][0:1, :], "ln1g")
            bcast_row(ln1b, I["ln1"][1:2, :], "ln1b")
            bcast_row(rbbc, I["rbias"][0:1, :], "rbbc")
            for t_, k_ in ((ebase, "ebase"), (eidx1, "eidx1"), (gatt, "gatt_l")):
                DMA(lambda e: e.dma_start(out=t_[:], in_=I[k_]), w=[k_])
            V(lambda e: e.memset(cntbc[:], 0.0), w=["cntbc"])
            DMA(lambda e: e.dma_start(out=wrb[:], in_=I["w_router"].rearrange("(kc p) n -> p kc n", p=128)), w=["wrb"], q="pool")
            DMA(lambda e: e.dma_start(out=wsg[:], in_=I["w_sg"].rearrange("(kc p) n -> p kc n", p=128)), w=["wsg"], q="pool")
            DMA(lambda e: e.dma_start(out=wsu[:], in_=I["w_su"].rearrange("(kc p) n -> p kc n", p=128)), w=["wsu"], q="pool")
            DMA(lambda e: e.dma_start(out=wsd[:], in_=I["w_sd"].rearrange("(fc p) n -> p fc n", p=128)), w=["wsd"], q="pool")
            wov = I["w_out"].rearrange("(kc p) n -> p kc n", p=128)
            for kc in range(8):
                DMA(lambda e: e.dma_start(out=wst[0][:], in_=wov[:, kc, :]), w=["wst0"])
                V(lambda e: e.tensor_tensor(out=woutg[:, kc, :], in0=wst[0][:], in1=g1bc[:], op=ALU.mult), r=["wst0", "g1bc"], w=["woutg"])
            sqa = P_("sqa", [128, 4, 512], BF16)
            matts = [P_("matt%d" % i, [128, 4, 512], BF16) for i in range(2)]
            rsa = P_("rsa", [128, 512])
            xt3 = [P_("x3t%d" % i, [128, D]) for i in range(2)]
            xr = P_("xr", [128, D])
            x1t = [P_("x1t%d" % i, [128, D]) for i in range(2)]
            h2f = P_("h2f", [128, D])
            h2b = [P_("h2b%d" % i, [128, D], BF16) for i in range(2)]
            h2Ts = [P_("h2T%d" % i, [128, 8, 128], BF16) for i in range(2)]
            st6, mv, rstd = P_("st6", [128, 2, 6]), P_("mv", [128, 2]), P_("rstd", [128, 1])
            rt = {n_: P_("r_" + n_, [128, NEXP]) for n_ in ("s", "biased", "masked", "sel", "wsel", "Gd", "destf", "junk", "eidsel")}
            selb = P_("selb", [128, NEXP], BF16)
            m8, gs, gsort, gok, pen = P_("m8", [128, 8, 8]), P_("gs", [128, 8]), P_("gsort", [128, 8]), P_("gok", [128, 8]), P_("pen", [128, 8])
            t8, v8, dk, dsum = P_("t8", [128, 8]), P_("v8", [128, 8]), P_("dk", [128, 8]), P_("dsum", [128, 1])
            hidT, sgs = P_("hidT", [128, 2, 128], BF16), P_("sgs", [128, 2, 128])
            shs = [P_("shs%d" % i, [128, D]) for i in range(2)]
            mixps = ps("mixps", [128, D], ctx=ph)
            shps = ps("shps", [128, D], ctx=ph)
            h2Tps = ps("h2Tps", [128, 8, 128], BF16, ctx=ph)
            rps = ps("rps", [128, 512], ctx=ph)
            cumps = ps("cumps", [128, 512], ctx=ph)
            hsps = ps("hsps", [128, 4, 128], ctx=ph)
            def grp(gp):
                g0 = gp * 512
                matt = matts[gp % 2]
                mn = "matt%d" % (gp % 2)
                for c in range(4):
                    A(lambda e: e.activation(out=sqa[:, c, :], in_=attT[:, c, g0:g0 + 512], func=AF.Square), r=["attT"], w=["sqa"])
                for c in range(4):
                    PE(lambda e: e.matmul(hsps[:].rearrange("p a b -> p (a b)"), lhsT=onesb[:], rhs=sqa[:, c, :], start=(c == 0), stop=(c == 3)),
                       r=["onesb", "sqa"], w=["hsps"], sig=(c == 3))
                V(lambda e: e.tensor_scalar(out=rsa[:], in0=hsps[:].rearrange("p a b -> p (a b)"), scalar1=1.0 / 512, scalar2=EPS, op0=ALU.mult, op1=ALU.add),
                  r=["hsps"], w=["rsa"])
                A(lambda e: e.activation(out=rsa[:], in_=rsa[:], func=AF.Sqrt), r=["rsa"], w=["rsa"])
                V(lambda e: e.reciprocal(out=rsa[:], in_=rsa[:]), r=["rsa"], w=["rsa"])
                for c in range(4):
                    V(lambda e: e.scalar_tensor_tensor(out=matt[:, c, :], in0=attT[:, c, g0:g0 + 512], scalar=gatt[:, c:c + 1], in1=rsa[:],
                                                       op0=ALU.mult, op1=ALU.mult), r=["attT", "gatt_l", "rsa"], w=[mn])

            def S1(ti):
                gp, sub = ti // 4, ti % 4
                matt = matts[gp % 2]
                mn = "matt%d" % (gp % 2)
                h2T = h2Ts[ti % 2]
                h2Tn = "h2T%d" % (ti % 2)
                tok0 = ti * 128
                k2 = ti % 2
                x_, x1_, hb_, sh_ = xt3[k2], x1t[k2], h2b[k2], shs[k2]
                xn_, x1n, hbn, shn = "x3t%d" % k2, "x1t%d" % k2, "h2b%d" % k2, "shs%d" % k2
                DMA(lambda e: e.dma_start(out=x_[:], in_=I["xs"][OWN + tok0:OWN + tok0 + 128, :]), w=[xn_])
                for nh in range(2):
                    for kc in range(8):
                        lh = matt[:, kc, sub * 128:(sub + 1) * 128] if kc < 4 else ssmn[:, kc - 4, tok0:tok0 + 128]
                        PE(lambda e: e.matmul(mixps[:, nh * 512:(nh + 1) * 512], lhsT=lh, rhs=woutg[:, kc, nh * 512:(nh + 1) * 512],
                                              start=(kc == 0), stop=(kc == 7)), r=[mn, "ssmn", "woutg"], w=["mixps"], sig=(kc == 7 and nh == 1))
                V(lambda e: e.scalar_tensor_tensor(out=xr[:], in0=x_[:], scalar=ALPHA, in1=mixps[:], op0=ALU.mult, op1=ALU.add),
                  r=[xn_, "mixps"], w=["xr"])
                layer_norm(xr, "xr", x1_, x1n, ln1g, ln1b, "ln1g", "ln1b", st6, mv, rstd)
                DMA(lambda e: e.dma_start(out=R["X1"][tok0:tok0 + 128, :], in_=x1_[:]), r=[x1n], w=["X1"], q="pool")
                V(lambda e: e.tensor_tensor(out=h2f[:], in0=x1_[:], in1=sc2[:], op=ALU.mult), r=[x1n, "sc2"], w=["h2f"])
                V(lambda e: e.tensor_tensor(out=hb_[:], in0=h2f[:], in1=sh2[:], op=ALU.add), r=["h2f", "sh2"], w=[hbn])
                for kc in range(8):
                    PE(lambda e: e.transpose(h2Tps[:, kc, :], hb_[:, kc * 128:(kc + 1) * 128], identb[:]), r=[hbn, "identb"], w=["h2Tps"])
                A(lambda e: e.activation(out=h2T[:].rearrange("p a b -> p (a b)"), in_=h2Tps[:].rearrange("p a b -> p (a b)"), func=AF.Copy),
                  r=["h2Tps"], w=[h2Tn])

            def S2(ti):
                tok0 = ti * 128
                k2 = ti % 2
                hb_, sh_ = h2b[k2], shs[k2]
                hbn, shn = "h2b%d" % k2, "shs%d" % k2
                h2T = h2Ts[ti % 2]
                h2Tn = "h2T%d" % (ti % 2)
                for kc in range(8):
                    PE(lambda e: e.matmul(rps[:, 0:NEXP], lhsT=h2T[:, kc, :], rhs=wrb[:, kc, :], start=(kc == 0), stop=(kc == 7)),
                       r=[h2Tn, "wrb"], w=["rps"], sig=(kc == 7))
                A(lambda e: e.activation(out=rt["s"][:], in_=rps[:, 0:NEXP], func=AF.Sigmoid), r=["rps"], w=["r_s"])
                V(lambda e: e.tensor_tensor(out=rt["biased"][:], in0=rt["s"][:], in1=rbbc[:], op=ALU.add), r=["r_s", "rbbc"], w=["r_biased"])
                for g_ in range(8):
                    V(lambda e: e.max(out=m8[:, g_, :], in_=rt["biased"][:, g_ * 32:(g_ + 1) * 32]), r=["r_biased"], w=["m8"])
                V(lambda e: e.tensor_tensor(out=gs[:], in0=m8[:, :, 0], in1=m8[:, :, 1], op=ALU.add), r=["m8"], w=["gs"])
                V(lambda e: e.max(out=gsort[:], in_=gs[:]), r=["gs"], w=["gsort"])
                V(lambda e: e.tensor_scalar(out=gok[:], in0=gs[:], scalar1=gsort[:, 3:4], scalar2=None, op0=ALU.is_ge), r=["gs", "gsort"], w=["gok"])
                V(lambda e: e.tensor_scalar(out=pen[:], in0=gok[:], scalar1=-1.0, scalar2=1e9, op0=ALU.add, op1=ALU.mult), r=["gok"], w=["pen"])
                V(lambda e: e.tensor_tensor(out=rt["masked"][:].rearrange("p (g e) -> p g e", g=8), in0=rt["biased"][:].rearrange("p (g e) -> p g e", g=8),
                                            in1=pen[:].unsqueeze(2).to_broadcast([128, 8, 32]), op=ALU.add), r=["r_biased", "pen"], w=["r_masked"])
                V(lambda e: e.max(out=t8[:], in_=rt["masked"][:]), r=["r_masked"], w=["t8"])
                V(lambda e: e.tensor_scalar(out=rt["sel"][:], in0=rt["masked"][:], scalar1=t8[:, 7:8], scalar2=None, op0=ALU.is_ge), r=["r_masked", "t8"], w=["r_sel"])
                V(lambda e: e.tensor_tensor(out=rt["wsel"][:], in0=rt["sel"][:], in1=rt["s"][:], op=ALU.mult), r=["r_sel", "r_s"], w=["r_wsel"])
                V(lambda e: e.reduce_sum(out=dsum[:], in_=rt["wsel"][:], axis=mybir.AxisListType.X), r=["r_wsel"], w=["dsum"])
                V(lambda e: e.reciprocal(out=dsum[:], in_=dsum[:]), r=["dsum"], w=["dsum"])
                V(lambda e: e.tensor_scalar(out=rt["Gd"][:], in0=rt["wsel"][:], scalar1=dsum[:, 0:1], scalar2=2.5, op0=ALU.mult, op1=ALU.mult), r=["r_wsel", "dsum"], w=["r_Gd"])
                A(lambda e: e.activation(out=selb[:], in_=rt["sel"][:], func=AF.Copy), r=["r_sel"], w=["selb"])
                PE(lambda e: e.matmul(cumps[:, 0:NEXP], lhsT=ltrib[:], rhs=selb[:], start=True, stop=True), r=["ltrib", "selb"], w=["cumps"], sig=False)
                PE(lambda e: e.matmul(cumps[:, NEXP:2 * NEXP], lhsT=onesb[:], rhs=selb[:], start=True, stop=True), r=["onesb", "selb"], w=["cumps"])
                V(lambda e: e.tensor_tensor(out=rt["destf"][:], in0=cumps[:, 0:NEXP], in1=cntbc[:], op=ALU.add), r=["cumps", "cntbc"], w=["r_destf"])
                V(lambda e: e.tensor_tensor(out=cntbc[:], in0=cumps[:, NEXP:2 * NEXP], in1=cntbc[:], op=ALU.add), r=["cumps", "cntbc", "r_destf"], w=["cntbc"])
                V(lambda e: e.tensor_tensor(out=rt["eidsel"][:], in0=rt["sel"][:], in1=eidx1[:], op=ALU.mult), r=["r_sel", "eidx1"], w=["r_eidsel"])
                V(lambda e: e.max(out=v8[:], in_=rt["eidsel"][:]), r=["r_eidsel"], w=["v8"])
                for k in range(8):
                    V(lambda e: e.scalar_tensor_tensor(out=rt["junk"][:], in0=rt["eidsel"][:], scalar=v8[:, k:k + 1], in1=rt["destf"][:],
                                                       op0=ALU.is_equal, op1=ALU.mult, accum_out=dk[:, k:k + 1]), r=["r_eidsel", "v8", "r_destf"], w=["r_junk", "dk"])
                    V(lambda e: e.scalar_tensor_tensor(out=rt["junk"][:], in0=rt["eidsel"][:], scalar=v8[:, k:k + 1], in1=rt["Gd"][:],
                                                       op0=ALU.is_equal, op1=ALU.mult, accum_out=GATE[:, ti * 8 + k:ti * 8 + k + 1]),
                      r=["r_eidsel", "v8", "r_Gd"], w=["r_junk", "GATE"])
                V(lambda e: e.tensor_copy(out=POS[:, ti * 8:(ti + 1) * 8], in_=dk[:]), r=["dk"], w=["POS"])
                V(lambda e: e.tensor_copy(out=V8S[:, ti * 8:(ti + 1) * 8], in_=v8[:]), r=["v8"], w=["V8S"])
                DMA(lambda e: e.dma_start(out=R["H2"][tok0:tok0 + 128, :], in_=hb_[:]), r=[hbn], w=["H2"], q="pool")
                for fc in range(2):
                    for gu, wt_, wtn in ((0, wsg, "wsg"), (1, wsu, "wsu")):
                        for kc in range(8):
                            PE(lambda e: e.matmul(hsps[:, fc * 2 + gu, :], lhsT=wt_[:, kc, fc * 128:(fc + 1) * 128], rhs=h2T[:, kc, :],
                                                  start=(kc == 0), stop=(kc == 7)), r=[wtn, h2Tn], w=["hsps"], sig=(kc == 7 and fc == 1 and gu == 1))
                for fc in range(2):
                    A(lambda e: e.activation(out=sgs[:, fc, :], in_=hsps[:, fc * 2, :], func=AF.Silu), r=["hsps"], w=["sgs"])
                    V(lambda e: e.tensor_tensor(out=hidT[:, fc, :], in0=hsps[:, fc * 2 + 1, :], in1=sgs[:, fc, :], op=ALU.mult), r=["hsps", "sgs"], w=["hidT"])
                for nh in range(2):
                    for fc in range(2):
                        PE(lambda e: e.matmul(shps[:, nh * 512:(nh + 1) * 512], lhsT=hidT[:, fc, :], rhs=wsd[:, fc, nh * 512:(nh + 1) * 512],
                                              start=(fc == 0), stop=(fc == 1)), r=["hidT", "wsd"], w=["shps"], sig=(fc == 1 and nh == 1))
                A(lambda e: e.activation(out=sh_[:], in_=shps[:], func=AF.Copy), r=["shps"], w=[shn])
                DMA(lambda e: e.dma_start(out=R["SHs"][tok0:tok0 + 128, :], in_=sh_[:]), r=[shn], w=["SHs"], q="pool")

            n_t3 = glim * 4
            grp(0)
            S1(0)
            for ti in range(n_t3):
                if ti + 1 < n_t3:
                    if (ti + 1) % 4 == 0:
                        grp((ti + 1) // 4)
                    S1(ti + 1)
                S2(ti)
            I32_ = mybir.dt.int32
            nbi = P_("nbi", [128, NEXP], I32_)
            nb, incl, bs128, onesf = P_("nb", [128, NEXP]), P_("incl", [128, NEXP]), P_("bs128", [128, NEXP]), P_("onesf", [128, NEXP])
            bendT, pidx, bidx = P_("bendT", [128, 2]), P_("pidx", [128, 1]), P_("bidx", [128, NBLK])
            cmpb = P_("cmpb", [128, 2, NBLK], BF16)
            ebf = P_("ebf", [128, NBLK])
            DMA(lambda e: e.dma_start(out=pidx[:], in_=I["pidx"]), w=["pidx"])
            DMA(lambda e: e.dma_start(out=bidx[:], in_=I["bidx"]), w=["bidx"])
            V(lambda e: e.memset(onesf[:], 1.0), w=["onesf"])
            V(lambda e: e.tensor_scalar(out=nbi[:], in0=cntbc[:], scalar1=1.0 / BR, scalar2=(BR - 1.0) / BR - 0.5 + 0.5 / BR, op0=ALU.mult, op1=ALU.add), r=["cntbc"], w=["nbi"])
            V(lambda e: e.tensor_copy(out=nb[:], in_=nbi[:]), r=["nbi"], w=["nb"])
            V(lambda e: e.tensor_tensor_scan(out=incl[:], data0=onesf[:], data1=nb[:], initial=0.0, op0=ALU.mult, op1=ALU.add), r=["onesf", "nb"], w=["incl"])
            V(lambda e: e.tensor_tensor(out=bs128[:], in0=incl[:], in1=nb[:], op=ALU.subtract), r=["incl", "nb"], w=["bs128"])
            V(lambda e: e.tensor_scalar(out=bs128[:], in0=bs128[:], scalar1=float(BR), scalar2=None, op0=ALU.mult), r=["bs128"], w=["bs128"])
            for c in range(2):
                PE(lambda e: e.transpose(rps[:, c * 128:(c + 1) * 128], incl[:, c * 128:(c + 1) * 128], ident[:]), r=["incl", "ident"], w=["rps"])
            V(lambda e: e.tensor_copy(out=bendT[:], in_=rps[:, 0:256:128]), r=["rps"], w=["bendT"])
            for c in range(2):
                V(lambda e: e.tensor_scalar(out=cmpb[:, c, :], in0=bidx[:], scalar1=bendT[:, c:c + 1], scalar2=None, op0=ALU.is_ge), r=["bidx", "bendT"], w=["cmpb"])
            for c in range(2):
                PE(lambda e: e.matmul(cumps[:, 0:NBLK], lhsT=onesb[:], rhs=cmpb[:, c, :], start=(c == 0), stop=(c == 1)), r=["onesb", "cmpb"], w=["cumps"], sig=(c == 1))
            V(lambda e: e.tensor_scalar(out=ebf[:], in0=cumps[:, 0:NBLK], scalar1=128.0, scalar2=None, op0=ALU.mult), r=["cumps"], w=["ebf"])
            V(lambda e: e.tensor_scalar(out=IDXW[:], in0=ebf[:], scalar1=pidx[:, 0:1], scalar2=None, op0=ALU.add), r=["ebf", "pidx"], w=["IDXW"])
            for ti in range(glim * 4):
                hb_, hbn = h2b[ti % 2], "h2b%d" % (ti % 2)
                DMA(lambda e: e.dma_start(out=hb_[:], in_=R["H2"][ti * 128:(ti + 1) * 128, :]), r=["H2"], w=[hbn])
                for k in range(8):
                    V(lambda e: e.scalar_tensor_tensor(out=rt["junk"][:], in0=eidx1[:], scalar=V8S[:, ti * 8 + k:ti * 8 + k + 1], in1=bs128[:],
                                                       op0=ALU.is_equal, op1=ALU.mult, accum_out=dk[:, k:k + 1]), r=["eidx1", "V8S", "bs128"], w=["r_junk", "dk"])
                V(lambda e: e.tensor_tensor(out=dk[:], in0=dk[:], in1=POS[:, ti * 8:(ti + 1) * 8], op=ALU.add), r=["dk", "POS"], w=["dk"])
                V(lambda e: e.tensor_copy(out=DEST[:, ti * 8:(ti + 1) * 8], in_=dk[:]), r=["dk"], w=["DEST%d" % ti])
                for k in range(8):
                    DMA(lambda e: e.indirect_dma_start(out=R["XSs"], out_offset=bass.IndirectOffsetOnAxis(ap=DEST[:, ti * 8 + k:ti * 8 + k + 1], axis=0),
                                                       in_=hb_[:], in_offset=None), r=[hbn, "DEST%d" % ti], w=["XSs_sc%d_%d" % (ti, k)], q="pool")
            if "x1" in dbg:
                dd6 = dbg_dram("dest", [128, 256], U32)
                DMA(lambda e: e.dma_start(out=dd6[:, 0:glim * 32], in_=DEST[:, 0:glim * 32]), r=["DEST%d" % t_ for t_ in range(glim * 4)], w=["dbgdest"])
                dd7 = dbg_dram("idxw", [128, NBLK], U32)
                DMA(lambda e: e.dma_start(out=dd7, in_=IDXW[:]), r=["IDXW"], w=["dbgidxw"])
                dd = dbg_dram("x1", [OWN, D])
                DMA(lambda e: e.dma_start(out=dd[0:glim * 512, :], in_=R["X1"][0:glim * 512, :]), r=["X1"], w=["dbgx1"])
                dd2 = dbg_dram("v8s", [128, 256])
                DMA(lambda e: e.dma_start(out=dd2[:, 0:glim * 32], in_=V8S[:, 0:glim * 32]), r=["V8S"], w=["dbgv8s"])
                dd3 = dbg_dram("gate", [128, 256])
                DMA(lambda e: e.dma_start(out=dd3[:, 0:glim * 32], in_=GATE[:, 0:glim * 32]), r=["GATE"], w=["dbggate"])
                dd4 = dbg_dram("shs", [OWN, D])
                DMA(lambda e: e.dma_start(out=dd4[0:glim * 512, :], in_=R["SHs"][0:glim * 512, :]), r=["SHs"], w=["dbgshs"])
                dd5 = dbg_dram("cnt", [128, NEXP])
                DMA(lambda e: e.dma_start(out=dd5, in_=cntbc[:]), r=["cntbc"], w=["dbgcnt"])
            S.barrier()
        ph23.close()
        if stage == 4:
            return finish(nc, S, out, dbg_out, I)


        with ExitStack() as ph:
            P_ = lambda name, shape, dt=F32: sb(name, shape, dt, ctx=ph)
            NW = 3
            wgb = [P_("wgb%d" % i, [128, 8, 256], BF16) for i in range(NW)]
            wub = [P_("wub%d" % i, [128, 8, 256], BF16) for i in range(NW)]
            wdb = [P_("wdb%d" % i, [128, 2, D], BF16) for i in range(NW)]
            NX = 3
            Xb = [P_("Xb%d" % i, [128, 2, D], BF16) for i in range(NX)]
            Yb = [P_("Yb%d" % i, [128, 2, D], BF16) for i in range(2)]
            xT = [P_("xT%d" % i, [128, 8, BR], BF16) for i in range(2)]
            sg4 = P_("sg4", [128, 2, BR])
            hT4 = [P_("hT4_%d" % i, [128, 2, BR], BF16) for i in range(2)]
            xTp = ps("xTp", [128, 8, BR], BF16, ctx=ph)
            hps = ps("hps", [128, 4, BR], ctx=ph)
            yps = [ps("yps4_%d" % i, [128, D], ctx=ph) for i in range(2)]
            wgv = I["w_eg"].rearrange("e (p kc) f -> (e p) (kc f)", kc=8)
            wuv = I["w_eu"].rearrange("e (p kc) f -> (e p) (kc f)", kc=8)
            wdv = I["w_ed"].rearrange("e (p fc) n -> (e p) (fc n)", fc=2)

            bc_reg = nc.gpsimd.to_reg(NEXP * 128 - 1)

            def stW(b):
                k3 = b % NW
                off = bass.IndirectOffsetOnAxis(ap=IDXW[:, b:b + 1], axis=0)
                for wt_, wv_, wn_ in ((wgb[k3], wgv, "wgb%d" % k3), (wub[k3], wuv, "wub%d" % k3), (wdb[k3], wdv, "wdb%d" % k3)):
                    DMA(lambda e: e.indirect_dma_start(out=wt_[:].rearrange("p a b -> p (a b)"), out_offset=None, in_=wv_, in_offset=off,
                                                       bounds_check=bc_reg, oob_is_err=False),
                        r=["IDXW"], w=[wn_], q="pool")

            def stX(b):
                X_, Xn = Xb[b % NX], "Xb%d" % (b % NX)
                DMA(lambda e: e.dma_start(out=X_[:], in_=R["XSs"][b * BR:(b + 1) * BR, :].rearrange("(r p) f -> p r f", p=128)), r=["XSs"], w=[Xn])

            def stT(b):
                k2 = b % 2
                X_, Xn = Xb[b % NX], "Xb%d" % (b % NX)
                for rt_ in range(2):
                    for kc in range(8):
                        PE(lambda e: e.transpose(xTp[:, kc, rt_ * 128:(rt_ + 1) * 128], X_[:, rt_, kc:D:8], identb[:]), r=[Xn, "identb"], w=["xTp"])
                A(lambda e: e.activation(out=xT[k2][:, 0:4, :], in_=xTp[:, 0:4, :], func=AF.Copy), r=["xTp"], w=["xT%d" % k2])
                V(lambda e: e.tensor_copy(out=xT[k2][:, 4:8, :], in_=xTp[:, 4:8, :]), r=["xTp"], w=["xT%d" % k2])

            def stGU(b):
                k2, k3 = b % 2, b % NW
                for fc in range(2):
                    for gu, wt_, wn_ in ((0, wgb[k3], "wgb%d" % k3), (1, wub[k3], "wub%d" % k3)):
                        for kc in range(8):
                            PE(lambda e: e.matmul(hps[:, fc * 2 + gu, :], lhsT=wt_[:, kc, fc:256:2], rhs=xT[k2][:, kc, :], start=(kc == 0), stop=(kc == 7)),
                               r=[wn_, "xT%d" % k2], w=["hps"], sig=(kc == 7 and fc == 1 and gu == 1))
                A(lambda e: e.activation(out=sg4[:], in_=hps[:, 0:4:2, :], func=AF.Silu), r=["hps"], w=["sg4"])
                V(lambda e: e.tensor_tensor(out=hT4[k2][:], in0=hps[:, 1:4:2, :], in1=sg4[:], op=ALU.mult), r=["hps", "sg4"], w=["hT4_%d" % k2])

            def stD(b):
                k2, k3 = b % 2, b % NW
                Y_, Yn = Yb[k2], "Yb%d" % k2
                for rt_ in range(2):
                    yp, ypn = yps[rt_], "yps4_%d" % rt_
                    for nh in range(2):
                        for fc in range(2):
                            PE(lambda e: e.matmul(yp[:, nh * 512:(nh + 1) * 512], lhsT=hT4[k2][:, fc, rt_ * 128:(rt_ + 1) * 128],
                                                  rhs=wdb[k3][:, fc, nh * 512:(nh + 1) * 512], start=(fc == 0), stop=(fc == 1)),
                               r=["hT4_%d" % k2, "wdb%d" % k3], w=[ypn], sig=(fc == 1 and nh == 1))
                    A(lambda e: e.activation(out=Y_[:, rt_, 0:512], in_=yp[:, 0:512], func=AF.Copy), r=[ypn], w=[Yn + "_%d" % rt_])
                    V(lambda e: e.tensor_copy(out=Y_[:, rt_, 512:D], in_=yp[:, 512:D]), r=[ypn], w=[Yn + "_%d" % rt_])
                DMA(lambda e: e.dma_start(out=R["YSs"][b * BR:(b + 1) * BR, :].rearrange("(r p) f -> p r f", p=128), in_=Y_[:]),
                    r=[Yn + "_0", Yn + "_1"], w=["YSs_%d" % b])

            for b0 in range(min(NW, blim)):
                stW(b0)
            for b0 in range(min(NX - 1, blim)):
                stX(b0)
            stT(0)
            for b in range(blim):
                if b + NX - 1 < blim:
                    stX(b + NX - 1)
                if b + 1 < blim:
                    stT(b + 1)
                stGU(b)
                if b >= 1:
                    stD(b - 1)
                if b >= 1 and b + 2 < blim:
                    stW(b + 2)
            stD(blim - 1)
            S.barrier()
        if stage == 5:
            return finish(nc, S, out, dbg_out, I)

        with ExitStack() as ph:
            P_ = lambda name, shape, dt=F32: sb(name, shape, dt, ctx=ph)
            g2bc, ln2g, ln2b = P_("g2bc", [128, D]), P_("ln2g", [128, D]), P_("ln2b", [128, D])
            bcast_row(g2bc, R["ADA"][3:4, :], "g2bc")
            bcast_row(ln2g, I["ln2"][0:1, :], "ln2g")
            bcast_row(ln2b, I["ln2"][1:2, :], "ln2b")
            Gk = [P_("Gk%d" % i, [128, D], BF16) for i in range(12)]
            sht = [P_("sht%d" % i, [128, D]) for i in range(3)]
            x1r = [P_("x1r%d" % i, [128, D]) for i in range(3)]

            def ld5(ti):
                k3 = ti % 3
                DMA(lambda e: e.dma_start(out=sht[k3][:], in_=R["SHs"][ti * 128:(ti + 1) * 128, :]), r=["SHs"], w=["sht%d" % k3])
                DMA(lambda e: e.dma_start(out=x1r[k3][:], in_=R["X1"][ti * 128:(ti + 1) * 128, :]), r=["X1"], w=["x1r%d" % k3])
            ld5(0)
            ot = [P_("ot%d" % i, [128, D]) for i in range(2)]
            st6, mv, rstd = P_("st6b", [128, 2, 6]), P_("mvb", [128, 2]), P_("rstdb", [128, 1])
            gi = 0
            for ti in range(glim * 4):
                k2, k3 = ti % 2, ti % 3
                a_, an = sht[k3], "sht%d" % k3
                x_, xn_ = x1r[k3], "x1r%d" % k3
                o_, on_ = ot[k2], "ot%d" % k2
                if ti + 1 < glim * 4:
                    ld5(ti + 1)
                for k in range(8):
                    g_, gn = Gk[gi % 12], "Gk%d" % (gi % 12)
                    gi += 1
                    DMA(lambda e: e.indirect_dma_start(out=g_[:], out_offset=None, in_=R["YSs"],
                                                       in_offset=bass.IndirectOffsetOnAxis(ap=DEST[:, ti * 8 + k:ti * 8 + k + 1], axis=0)),
                        r=["YSs", "DEST"], w=[gn], q="pool")
                    V(lambda e: e.scalar_tensor_tensor(out=a_[:], in0=g_[:], scalar=GATE[:, ti * 8 + k:ti * 8 + k + 1], in1=a_[:], op0=ALU.mult, op1=ALU.add),
                      r=[gn, "GATE", an], w=[an])
                V(lambda e: e.tensor_tensor(out=a_[:], in0=a_[:], in1=g2bc[:], op=ALU.mult), r=[an, "g2bc"], w=[an])
                V(lambda e: e.scalar_tensor_tensor(out=a_[:], in0=x_[:], scalar=ALPHA, in1=a_[:], op0=ALU.mult, op1=ALU.add), r=[xn_, an], w=[an])
                layer_norm(a_, an, o_, on_, ln2g, ln2b, "ln2g", "ln2b", st6, mv, rstd)
                DMA(lambda e: e.dma_start(out=out[ti * 128:(ti + 1) * 128, :], in_=o_[:]), r=[on_], w=["out"])
            S.barrier()
        return finish(nc, S, out, dbg_out, I)


def finish(nc, S, out, dbg_out, I):
    S.barrier()
    return nc, dbg_out, list(I.keys())


_SHAPES = None


def kernel(**inputs):
    maps = host_prep(inputs)
    shapes = {k: v.shape for k, v in maps[0].items()}
    nc, _, used = build(shapes)
    maps = [{k: m[k] for k in used} for m in maps]
    res = run_bass_kernel_spmd(nc, maps, core_ids=list(range(NCORES)))
    outp = np.zeros((4, SEQ, D), np.float32)
    for ci in range(NCORES):
        b, half = ci // 2, ci % 2
        outp[b, half * OWN:(half + 1) * OWN] = res.results[ci]["out"]
    return outp
```

```python
import math
import numpy as np
import concourse.bass as bass
import concourse.mybir as mybir
from concourse.bass_utils import run_bass_kernel_spmd

F32 = mybir.dt.float32
BF16 = mybir.dt.bfloat16
U32 = mybir.dt.uint32
AF = mybir.ActivationFunctionType
ALU = mybir.AluOpType

D = 1024
SEQ = 8192
OWN = 4096
NCORES = 8
NEXP = 256
CAP = 256
CSTR = CAP
BR = 256
NBLK = 384
NROWS = NBLK * BR
ALPHA = 2.0 ** 0.25
EPS = 1e-5
PI = math.pi
DILS = (1, 4, 16)
NEGM = -30000.0


class Sched:
    def __init__(self, nc, n_lanes=8):
        self.nc = nc
        self.eng = {"pe": nc.tensor, "dve": nc.vector, "act": nc.scalar,
                    "pool": nc.gpsimd, "sp": nc.sync}
        self.sem = {}
        self.cnt = {}
        for e in ("pe", "dve", "act", "pool"):
            self.sem[e] = nc.alloc_semaphore("s_" + e)
            self.cnt[e] = 0
        self.lanes = {}
        for q in ("sp", "pool"):
            self.lanes[q] = []
            for i in range(n_lanes):
                k = "l_%s%d" % (q, i)
                self.sem[k] = nc.alloc_semaphore(k)
                self.cnt[k] = 0
                self.lanes[q].append(k)
        self.lane_rr = {q: 0 for q in self.lanes}
        self.seen = {e: {} for e in self.eng}
        self.last_w = {}
        self.readers = {}
        self.nops = 0

    def _deps(self, reads, writes):
        deps = {}

        def add(t):
            if t is not None and deps.get(t[0], 0) < t[1]:
                deps[t[0]] = t[1]
        for r in reads:
            add(self.last_w.get(r))
        for w in writes:
            add(self.last_w.get(w))
            for t in self.readers.get(w, ()):
                add(t)
        return deps

    def _wait(self, e, deps):
        seen = self.seen[e]
        for k, v in deps.items():
            if e == "pe" and k == "pe":
                continue
            if seen.get(k, 0) >= v:
                continue
            self.eng[e].wait_ge(self.sem[k], v)
            seen[k] = v

    def _record(self, ticket, reads, writes):
        for r in reads:
            lst = self.readers.setdefault(r, [])
            lst.append(ticket)
            if len(lst) > 64:
                best = {}
                for k, v in lst:
                    if best.get(k, 0) < v:
                        best[k] = v
                self.readers[r] = list(best.items())
        for w in writes:
            self.last_w[w] = ticket
            self.readers[w] = []

    def op(self, e, fn, reads=(), writes=(), sig=True):
        self._wait(e, self._deps(reads, writes))
        inst = fn(self.eng[e])
        if sig:
            self.cnt[e] += 1
            inst.then_inc(self.sem[e], 1)
            ticket = (e, self.cnt[e])
        else:
            ticket = (e, self.cnt[e] + 1)
        self._record(ticket, reads, writes)
        self.nops += 1
        return inst

    def dma(self, q, fn, reads=(), writes=()):
        lane = self.lanes[q][self.lane_rr[q] % len(self.lanes[q])]
        self.lane_rr[q] += 1
        deps = self._deps(reads, writes)
        if self.cnt[lane] > 0:
            deps[lane] = max(deps.get(lane, 0), 16 * self.cnt[lane])
        self._wait(q, deps)
        inst = fn(self.eng[q])
        self.cnt[lane] += 1
        inst.then_inc(self.sem[lane], 16)
        self._record((lane, 16 * self.cnt[lane]), reads, writes)
        self.nops += 1
        return inst

    def barrier(self):
        final = {}
        for k, c in self.cnt.items():
            if c > 0:
                final[k] = c * (16 if k.startswith("l_") else 1)
        for e in self.eng:
            self._wait(e, dict(final))
        self.last_w = {}
        self.readers = {}


def _t5_bucket(dist):
    exact = 16
    d = np.maximum(dist, 1).astype(np.float32)
    large = exact + (np.log(d / np.float32(exact)) / np.float32(math.log(2048 / exact))
                     * np.float32(32 - exact)).astype(np.int32)
    return np.where(dist < exact, dist, np.minimum(large, 31)).astype(np.int64)


def _bias_layout(rel_bias):
    i = np.arange(128)[:, None]
    j = np.arange(128)[None, :]
    tb = np.zeros((128, 3, 8, 2, 128), np.float32)
    mk = np.zeros((128, 2, 128), np.float32)
    for kt in range(2):
        rel = (j - i) if kt == 1 else (j + 128 - i)
        ok = (rel >= 0) & (rel <= 128)
        mk[:, kt, :] = np.where(ok, 0.0, NEGM)
        for pi, d in enumerate(DILS):
            bk = _t5_bucket(np.maximum(rel, 0) * d)
            for h in range(8):
                tb[:, pi, h, kt, :] = np.where(ok, rel_bias[bk, h], 0.0)
    return tb.reshape(128, 48, 128), mk


def _ssm_layout(a):
    return np.ascontiguousarray(a.reshape(16, 2, 64).transpose(1, 2, 0).reshape(128, 16))


def host_prep(inp):
    f = lambda a: np.ascontiguousarray(a, dtype=np.float32)
    sh = {}
    sh["w_ada"] = f(inp["w_ada"][0])
    sh["b_ada"] = f(inp["b_ada"][0]).reshape(1, 6 * D)
    sh["b_col"] = f(inp["b_ada"][0][:2048].reshape(16, 128).T)
    sh["w_in"] = f(inp["w_in"][0])
    sh["a_re"] = _ssm_layout(f(inp["ssm_a_re"][0]))
    sh["a_im"] = _ssm_layout(f(inp["ssm_a_im"][0]))
    sh["ldt"] = _ssm_layout(np.repeat(f(inp["ssm_log_dt"][0])[:, None], 64, axis=1))
    bl = lambda b: np.ascontiguousarray(b.reshape(16, 2, 64, 16).transpose(1, 2, 0, 3).reshape(128, 256))
    sh["b_re"] = bl(f(inp["ssm_b_re"][0]))
    sh["b_im"] = bl(f(inp["ssm_b_im"][0]))
    cl = lambda c: np.ascontiguousarray(c.reshape(16, 2, 16, 64).transpose(1, 3, 0, 2).reshape(128, 256))
    sh["c_re"] = cl(f(inp["ssm_c_re"][0]))
    sh["c_im"] = cl(f(inp["ssm_c_im"][0]))
    sh["d_l"] = f(inp["ssm_d"][0].reshape(4, 128).T)
    sh["w_glu"] = f(inp["w_glu"][0])
    sh["bglu_l"] = f(inp["b_glu"][0].reshape(4, 128).T)
    sh["gatt_l"] = f(inp["g_att"][0].reshape(4, 128).T)
    sh["gssm_l"] = f(inp["g_ssm"][0].reshape(4, 128).T)
    sh["w_out"] = f(inp["w_out"][0])
    sh["ln1"] = f(np.stack([inp["ln1_g"][0], inp["ln1_b"][0]]))
    sh["ln2"] = f(np.stack([inp["ln2_g"][0], inp["ln2_b"][0]]))
    sh["w_router"] = f(inp["w_router"][0])
    sh["rbias"] = f(inp["router_bias"][0]).reshape(1, NEXP)
    sh["w_eg"] = f(inp["w_e_gate"][0])
    sh["w_eu"] = f(inp["w_e_up"][0])
    sh["w_ed"] = f(inp["w_e_down"][0])
    sh["w_sg"] = f(inp["w_s_gate"][0])
    sh["w_su"] = f(inp["w_s_up"][0])
    sh["w_sd"] = f(inp["w_s_down"][0])
    tb, mk = _bias_layout(f(inp["rel_bias"]))
    sh["tb"] = tb
    sh["mk"] = mk
    sh["ident"] = np.eye(128, dtype=np.float32)
    sh["ltri"] = np.triu(np.ones((128, 128), np.float32), 1)
    sh["mask2"] = np.stack([(np.arange(128) < 64), (np.arange(128) >= 64)], 1).astype(np.float32)
    sh["jidx"] = np.tile(np.arange(512, dtype=np.float32)[None, :], (128, 1))
    sh["ebase"] = np.tile((np.arange(NEXP, dtype=np.float32) * CSTR)[None, :], (128, 1))
    sh["pidx"] = np.arange(128, dtype=np.float32).reshape(128, 1)
    sh["bidx"] = np.tile(np.arange(NBLK, dtype=np.float32)[None, :], (128, 1))
    sh["eidx1"] = np.tile((np.arange(NEXP, dtype=np.float32) + 1.0)[None, :], (128, 1))
    x = f(inp["x"])
    c = f(inp["c"])
    maps = []
    for ci in range(NCORES):
        b, half = ci // 2, ci % 2
        m = dict(sh)
        xs = np.zeros((SEQ, D), np.float32)
        if half == 1:
            xs[:] = x[b]
        else:
            xs[OWN:] = x[b, :OWN]
        m["xs"] = xs
        m["ccol"] = np.ascontiguousarray(c[b].reshape(8, 128).T)
        m["flag"] = np.full((128, 1), float(half), np.float32)
        maps.append(m)
    return maps


def build(shapes, stage=99, dbg=(), lim=16, alim=4, glim=8, blim=NBLK):
    nc = bass.Bass("TRN2", target_bir_lowering=False)
    S = Sched(nc)
    class _Lazy(dict):
        def __missing__(self, k):
            self[k] = nc.dram_tensor(k, list(shapes[k]), F32, kind="ExternalInput").ap()
            return self[k]
    I = _Lazy()
    out = nc.dram_tensor("out", [OWN, D], F32, kind="ExternalOutput").ap()
    dbg_out = {}

    def dbg_dram(name, shape, dt=F32):
        t = nc.dram_tensor("dbg_" + name, list(shape), dt, kind="ExternalOutput").ap()
        dbg_out[name] = t
        return t

    _scr = {"UT": ([512, SEQ], BF16), "KT": ([512, 6144], BF16), "QT": ([512, OWN], BF16),
            "VS": ([6144, 512], BF16), "ADA": ([4, D], F32), "X1": ([OWN, D], F32),
            "SHs": ([OWN, D], F32), "H2": ([OWN, D], BF16), "XSs": ([NROWS, D], BF16), "YSs": ([NROWS, D], BF16)}

    class _LazyScr(dict):
        def __missing__(self, k):
            self[k] = nc.dram_tensor(k, _scr[k][0], _scr[k][1]).ap()
            return self[k]
    R = _LazyScr()

    V = lambda fn, r=(), w=(): S.op("dve", fn, r, w)
    A = lambda fn, r=(), w=(): S.op("act", fn, r, w)
    G = lambda fn, r=(), w=(): S.op("pool", fn, r, w)
    PE = lambda fn, r=(), w=(), sig=True: S.op("pe", fn, r, w, sig)
    DMA = lambda fn, r=(), w=(), q="sp": S.dma(q, fn, r, w)

    from contextlib import ExitStack
    with ExitStack() as top:
        sb = lambda name, shape, dt=F32, ctx=top: ctx.enter_context(nc.sbuf_tensor("s_" + name, list(shape), dt))
        ps = lambda name, shape, dt=F32, ctx=top: ctx.enter_context(nc.psum_tensor("p_" + name, list(shape), dt))

        ident = sb("ident", [128, 128])
        identb = sb("identb", [128, 128], BF16)
        onesb = sb("onesb", [128, 128], BF16)
        ltrib = sb("ltrib", [128, 128], BF16)
        flag = sb("flag", [128, 1])
        s1p = sb("s1p", [128, 8])
        sh1 = sb("sh1", [128, 8])
        DEST = sb("DEST", [128, 32 * 8], U32)
        GATE = sb("GATE", [128, 32 * 8])
        POS = sb("POS", [128, 32 * 8])
        V8S = sb("V8S", [128, 32 * 8])
        IDXW = sb("IDXW", [128, NBLK], U32)
        ssmn = sb("ssmn", [128, 4, OWN], BF16)

        DMA(lambda e: e.dma_start(out=ident[:], in_=I["ident"]), w=["ident"])
        DMA(lambda e: e.dma_start(out=flag[:], in_=I["flag"]), w=["flag"])
        DMA(lambda e: e.dma_start(out=identb[:], in_=I["ident"]), w=["identb"], q="pool")
        DMA(lambda e: e.dma_start(out=ltrib[:], in_=I["ltri"]), w=["ltrib"], q="pool")
        V(lambda e: e.memset(onesb[:], 1.0), w=["onesb"])

        with ExitStack() as ph:
            ccol = sb("ccol", [128, 8], ctx=ph)
            sc = sb("sc", [128, 8], ctx=ph)
            scb = sb("scb", [128, 8, 128], ctx=ph)
            bcol = sb("bcol", [128, 16], ctx=ph)
            bbc = sb("bbc", [128, 4096], ctx=ph)
            wt = [sb("wt%d" % i, [128, 8, 512], ctx=ph) for i in range(2)]
            adab = sb("adab", [128, 512], ctx=ph)
            pcol = ps("pcol", [128, 16], ctx=ph)
            prow = [ps("prow%d" % i, [128, 512], ctx=ph) for i in range(2)]
            DMA(lambda e: e.dma_start(out=ccol[:], in_=I["ccol"]), w=["ccol"])
            DMA(lambda e: e.dma_start(out=bcol[:], in_=I["b_col"]), w=["bcol"])
            DMA(lambda e: e.dma_start(out=bbc[:], in_=I["b_ada"][0:1, 2048:6144].to_broadcast([128, 4096])), w=["bbc"])
            A(lambda e: e.activation(out=sc[:], in_=ccol[:], func=AF.Silu), r=["ccol"], w=["sc"])
            V(lambda e: e.tensor_copy(out=scb[:], in_=sc[:].unsqueeze(2).to_broadcast([128, 8, 128])), r=["sc"], w=["scb"])
            wv = I["w_ada"].rearrange("(kc p) n -> p kc n", p=128)
            for ct in range(12):
                w_ = wt[ct % 2]
                wn = "wt%d" % (ct % 2)
                DMA(lambda e: e.dma_start(out=w_[:], in_=wv[:, :, ct * 512:(ct + 1) * 512]), w=[wn])
                if ct < 4:
                    for j in range(4):
                        col = ct * 4 + j
                        for kc in range(8):
                            PE(lambda e: e.matmul(pcol[:, col:col + 1], lhsT=w_[:, kc, j * 128:(j + 1) * 128],
                                                  rhs=sc[:, kc:kc + 1], start=(kc == 0), stop=(kc == 7)),
                               r=[wn, "sc"], w=["pcol"], sig=(kc == 7))
                else:
                    pr = prow[ct % 2]
                    pn = "prow%d" % (ct % 2)
                    for kc in range(8):
                        PE(lambda e: e.matmul(pr[:], lhsT=scb[:, kc, :], rhs=w_[:, kc, :], start=(kc == 0), stop=(kc == 7)),
                           r=[wn, "scb"], w=[pn], sig=(kc == 7))
                    c0 = (ct - 4) * 512
                    V(lambda e: e.tensor_tensor(out=adab[:], in0=pr[:], in1=bbc[:, c0:c0 + 512], op=ALU.add),
                      r=[pn, "bbc"], w=["adab"])
                    row = (ct - 4) // 2
                    if row == 2:
                        V(lambda e: e.tensor_scalar_add(out=adab[:], in0=adab[:], scalar1=1.0), r=["adab"], w=["adab"])
                    hc = ((ct - 4) % 2) * 512
                    DMA(lambda e: e.dma_start(out=R["ADA"][row:row + 1, hc:hc + 512], in_=adab[0:1, :]), r=["adab"], w=["ADA"])
            V(lambda e: e.tensor_tensor(out=sh1[:], in0=pcol[:, 0:8], in1=bcol[:, 0:8], op=ALU.add), r=["pcol", "bcol"], w=["sh1"])
            V(lambda e: e.scalar_tensor_tensor(out=s1p[:], in0=pcol[:, 8:16], scalar=1.0, in1=bcol[:, 8:16],
                                               op0=ALU.add, op1=ALU.add), r=["pcol", "bcol"], w=["s1p"])
            if "ada" in dbg:
                d1 = dbg_dram("ada_col", [128, 16])
                DMA(lambda e: e.dma_start(out=d1[:, 0:8], in_=sh1[:]), r=["sh1"], w=["dbg"])
                DMA(lambda e: e.dma_start(out=d1[:, 8:16], in_=s1p[:]), r=["s1p"], w=["dbg"])
                d2 = dbg_dram("ada_row", [4, D])
                t4 = sb("t4", [4, D], ctx=ph)
                DMA(lambda e: e.dma_start(out=t4[:], in_=R["ADA"]), r=["ADA"], w=["t4"])
                DMA(lambda e: e.dma_start(out=d2, in_=t4[:]), r=["t4"], w=["dbg2"])
            S.barrier()
        if stage == 0:
            return finish(nc, S, out, dbg_out, I)

        with ExitStack() as ph:
            winb = sb("winb", [128, 8, 2048], BF16, ctx=ph)
            xt = [sb("xt%d" % i, [128, D], ctx=ph) for i in range(3)]
            hT = [sb("hT%d" % i, [128, 8, 512], BF16, ctx=ph) for i in range(2)]
            ev = [sb("ev%d" % i, [128, 4, 512], BF16, ctx=ph) for i in range(4)]
            pT = [ps("pT%d" % i, [128, 1024], ctx=ph) for i in range(2)]
            pp = [ps("pp%d" % i, [128, 512], ctx=ph) for i in range(4)]
            wiv = I["w_in"].rearrange("(kc p) n -> p kc n", p=128)
            for kc in range(8):
                DMA(lambda e: e.dma_start(out=winb[:, kc, :], in_=wiv[:, kc, :]), w=["winb"], q="pool")
            xi = 0
            pi_ = 0
            def stA(st):
                nonlocal xi
                h_ = hT[st % 2]
                hn = "hT%d" % (st % 2)
                for sub in range(4):
                    ti = st * 4 + sub
                    x_ = xt[xi % 3]
                    xn = "xt%d" % (xi % 3)
                    p_ = pT[xi % 2]
                    pn = "pT%d" % (xi % 2)
                    xi += 1
                    DMA(lambda e: e.dma_start(out=x_[:], in_=I["xs"][ti * 128:(ti + 1) * 128, :]), w=[xn])
                    for kc in range(8):
                        PE(lambda e: e.transpose(p_[:, kc * 128:(kc + 1) * 128], x_[:, kc * 128:(kc + 1) * 128], ident[:]),
                           r=[xn, "ident"], w=[pn + "_b%d" % (kc // 4)])
                    for kc in range(8):
                        fn = lambda e: e.activation(out=h_[:, kc, sub * 128:(sub + 1) * 128], in_=p_[:, kc * 128:(kc + 1) * 128],
                                                    func=AF.Identity, scale=s1p[:, kc:kc + 1], bias=sh1[:, kc:kc + 1])
                        A(fn, r=[pn + "_b%d" % (kc // 4), "s1p", "sh1"], w=[hn + "_%d" % sub])
            def stB(st):
                nonlocal pi_
                h_ = hT[st % 2]
                hn = "hT%d" % (st % 2)
                hres = [hn + "_%d" % s_ for s_ in range(4)]
                jobs = [("u", 1536, R["UT"], st * 512, 1.0)]
                if st >= 4:
                    jobs.append(("k", 512, R["KT"], (st - 4) * 512, 1.0))
                if st >= 8:
                    jobs.append(("q", 0, R["QT"], (st - 8) * 512, 0.125))
                for ji, (nm, c0, dst, t0, scl) in enumerate(jobs):
                    e_ = ev[ji]
                    en = "ev%d" % ji
                    for oc in range(4):
                        pq = pp[pi_ % 4]
                        pqn = "pp%d" % (pi_ % 4)
                        pi_ += 1
                        for kc in range(8):
                            PE(lambda e: e.matmul(pq[:], lhsT=winb[:, kc, c0 + oc * 128:c0 + (oc + 1) * 128], rhs=h_[:, kc, :],
                                                  start=(kc == 0), stop=(kc == 7)), r=["winb"] + hres, w=[pqn], sig=(kc == 7))
                        if oc % 2 == 0:
                            A(lambda e: e.activation(out=e_[:, oc, :], in_=pq[:], func=AF.Copy, scale=scl), r=[pqn], w=[en])
                        else:
                            V(lambda e: e.tensor_scalar(out=e_[:, oc, :], in0=pq[:], scalar1=scl, scalar2=None, op0=ALU.mult),
                              r=[pqn], w=[en])
                    DMA(lambda e: e.dma_start(out=dst.rearrange("(oc p) t -> p oc t", p=128)[:, :, t0:t0 + 512], in_=e_[:]),
                        r=[en], w=[nm + "T"], q="pool")
                if st >= 4:
                    e_ = ev[3]
                    for sub in range(4):
                        pq = pp[pi_ % 4]
                        pqn = "pp%d" % (pi_ % 4)
                        pi_ += 1
                        for kc in range(8):
                            PE(lambda e: e.matmul(pq[:], lhsT=h_[:, kc, sub * 128:(sub + 1) * 128], rhs=winb[:, kc, 1024:1536],
                                                  start=(kc == 0), stop=(kc == 7)), r=["winb"] + hres, w=[pqn], sig=(kc == 7))
                        if sub % 2 == 0:
                            A(lambda e: e.activation(out=e_[:, sub, :], in_=pq[:], func=AF.Copy), r=[pqn], w=["ev3"])
                        else:
                            V(lambda e: e.tensor_copy(out=e_[:, sub, :], in_=pq[:]), r=[pqn], w=["ev3"])
                    r0 = (st - 4) * 512
                    DMA(lambda e: e.dma_start(out=R["VS"][r0:r0 + 512, :].rearrange("(s p) f -> p s f", p=128), in_=e_[:]),
                        r=["ev3"], w=["VS"], q="pool")
            n_st = 16 if lim >= 16 else lim
            stA(0)
            for st in range(n_st):
                if st + 1 < n_st:
                    stA(st + 1)
                stB(st)
            if "proj" in dbg:
                for nm, src, shp, rn in (("UT", R["UT"], [512, SEQ], "uT"), ("KT", R["KT"], [512, 6144], "kT"),
                                         ("QT", R["QT"], [512, OWN], "qT"), ("VS", R["VS"], [6144, 512], "VS")):
                    dd = dbg_dram(nm, shp, BF16)
                    DMA(lambda e: e.dma_start(out=dd, in_=src), r=[rn], w=["dbgo" + nm])
            S.barrier()
        if stage == 1:
            return finish(nc, S, out, dbg_out, I)


        with ExitStack() as ph:
            P_ = lambda name, shape, dt=F32: sb(name, shape, dt, ctx=ph)
            are, aim, ldt = P_("are", [128, 16]), P_("aim", [128, 16]), P_("ldt", [128, 16])
            bre, bim = P_("bre", [128, 16, 16]), P_("bim", [128, 16, 16])
            cre, cim = P_("cre", [128, 16, 16]), P_("cim", [128, 16, 16])
            mask2, jidx = P_("mask2", [128, 2]), P_("jidx", [128, 512])
            d_l, bglu, gssm = P_("d_l", [128, 4]), P_("bglu", [128, 4]), P_("gssm", [128, 4])
            wglub = P_("wglub", [128, 4, 512], BF16)
            for t_, k_ in ((are, "a_re"), (aim, "a_im"), (ldt, "ldt"), (mask2, "mask2"), (jidx, "jidx"),
                           (d_l, "d_l"), (bglu, "bglu_l"), (gssm, "gssm_l")):
                DMA(lambda e: e.dma_start(out=t_[:], in_=I[k_]), w=[k_])
            for t_, k_ in ((bre, "b_re"), (bim, "b_im"), (cre, "c_re"), (cim, "c_im")):
                DMA(lambda e: e.dma_start(out=t_[:].rearrange("p q c -> p (q c)"), in_=I[k_]), w=[k_])
            DMA(lambda e: e.dma_start(out=wglub[:], in_=I["w_glu"].rearrange("(cc p) n -> p cc n", p=128)), w=["wglub"], q="pool")
            sm = {n_: P_(n_, [128, 16]) for n_ in ("dt", "xre", "th", "rho", "m1", "sn", "cs", "abr", "abi", "nr",
                                                   "den", "t1", "t2", "cr", "ci", "c512", "s512", "glr", "gli",
                                                   "car", "cai", "u1", "u2")}
            tt = lambda o, a, b, op, r, w: V(lambda e: e.tensor_tensor(out=o, in0=a, in1=b, op=op), r=r, w=w)

            I32 = mybir.dt.int32
            C1, C2 = 6.28125, 2 * PI - 6.28125
            rk_i = P_("rk_i", [128, 512], I32)
            wk = [{n_: P_("%s%d" % (n_, i), [128, 512]) for n_ in ("ta", "tb", "wre", "wim", "gre", "gim")} for i in range(2)]
            rk_f, rk_r, rk_x = wk[1]["ta"], wk[1]["tb"], wk[1]["wre"]

            def sin_rr(out_ap, x_ap, x_res, n, w_res):
                ki, kf, r = rk_i[:, 0:n], rk_f[:, 0:n], rk_r[:, 0:n]
                V(lambda e: e.tensor_scalar(out=ki, in0=x_ap, scalar1=1.0 / (2 * PI), scalar2=None, op0=ALU.mult), r=x_res, w=["rk_i"])
                V(lambda e: e.tensor_copy(out=kf, in_=ki), r=["rk_i"], w=["rk_f"])
                V(lambda e: e.scalar_tensor_tensor(out=r, in0=kf, scalar=-C1, in1=x_ap, op0=ALU.mult, op1=ALU.add), r=["rk_f"] + x_res, w=["rk_r"])
                V(lambda e: e.scalar_tensor_tensor(out=r, in0=kf, scalar=-C2, in1=r, op0=ALU.mult, op1=ALU.add), r=["rk_f", "rk_r"], w=["rk_r"])
                V(lambda e: e.tensor_scalar(out=kf, in0=r, scalar1=PI, scalar2=-2 * PI, op0=ALU.is_gt, op1=ALU.mult), r=["rk_r"], w=["rk_f"])
                V(lambda e: e.tensor_tensor(out=r, in0=r, in1=kf, op=ALU.add), r=["rk_r", "rk_f"], w=["rk_r"])
                V(lambda e: e.tensor_scalar(out=r, in0=r, scalar1=-PI, scalar2=PI, op0=ALU.max, op1=ALU.min), r=["rk_r"], w=["rk_r"])
                A(lambda e: e.activation(out=out_ap, in_=r, func=AF.Sin), r=["rk_r"], w=w_res)

            def sincos(x_ap, x_res, sn_ap, cs_ap, n, sn_res, cs_res):
                V(lambda e: e.tensor_scalar_add(out=rk_x[:, 0:n], in0=x_ap, scalar1=0.5 * PI), r=x_res, w=["rk_x"])
                sin_rr(cs_ap, rk_x[:, 0:n], ["rk_x"], n, cs_res)
                sin_rr(sn_ap, x_ap, x_res, n, sn_res)

            A(lambda e: e.activation(out=sm["dt"][:], in_=ldt[:], func=AF.Exp), r=["ldt"], w=["dt"])
            tt(sm["xre"][:], sm["dt"][:], are[:], ALU.mult, ["dt", "a_re"], ["xre"])
            tt(sm["th"][:], sm["dt"][:], aim[:], ALU.mult, ["dt", "a_im"], ["th"])
            A(lambda e: e.activation(out=sm["rho"][:], in_=sm["xre"][:], func=AF.Exp), r=["xre"], w=["rho"])
            sincos(sm["th"][:], ["th"], sm["sn"][:], sm["cs"][:], 16, ["sc_sn"], ["sc_cs"])
            tt(sm["abr"][:], sm["rho"][:], sm["cs"][:], ALU.mult, ["rho", "sc_cs"], ["abr"])
            tt(sm["abi"][:], sm["rho"][:], sm["sn"][:], ALU.mult, ["rho", "sc_sn"], ["abi"])
            V(lambda e: e.tensor_scalar_add(out=sm["nr"][:], in0=sm["abr"][:], scalar1=-1.0), r=["abr"], w=["nr"])
            tt(sm["t1"][:], are[:], are[:], ALU.mult, ["a_re"], ["t1"])
            tt(sm["t2"][:], aim[:], aim[:], ALU.mult, ["a_im"], ["t2"])
            tt(sm["den"][:], sm["t1"][:], sm["t2"][:], ALU.add, ["t1", "t2"], ["den"])
            V(lambda e: e.reciprocal(out=sm["den"][:], in_=sm["den"][:]), r=["den"], w=["den"])
            tt(sm["t1"][:], sm["nr"][:], are[:], ALU.mult, ["nr", "a_re", "den"], ["t1"])
            tt(sm["t2"][:], sm["abi"][:], aim[:], ALU.mult, ["abi", "a_im", "den"], ["t2"])
            tt(sm["cr"][:], sm["t1"][:], sm["t2"][:], ALU.add, ["t1", "t2"], ["cr"])
            tt(sm["cr"][:], sm["cr"][:], sm["den"][:], ALU.mult, ["cr", "den"], ["cr"])
            tt(sm["t1"][:], sm["abi"][:], are[:], ALU.mult, ["abi", "a_re", "cr"], ["t1"])
            tt(sm["t2"][:], sm["nr"][:], aim[:], ALU.mult, ["nr", "a_im", "cr"], ["t2"])
            tt(sm["ci"][:], sm["t1"][:], sm["t2"][:], ALU.subtract, ["t1", "t2"], ["ci"])
            tt(sm["ci"][:], sm["ci"][:], sm["den"][:], ALU.mult, ["ci", "den"], ["ci"])
            bbr, bbi, tq = P_("bbr", [128, 16, 16]), P_("bbi", [128, 16, 16]), P_("tq", [128, 16, 16])
            crb = sm["cr"][:].unsqueeze(2).to_broadcast([128, 16, 16])
            cib = sm["ci"][:].unsqueeze(2).to_broadcast([128, 16, 16])
            tt(bbr[:], bre[:], crb, ALU.mult, ["b_re", "cr"], ["bbr"])
            tt(tq[:], bim[:], cib, ALU.mult, ["b_im", "ci"], ["tq"])
            tt(bbr[:], bbr[:], tq[:], ALU.subtract, ["bbr", "tq"], ["bbr"])
            tt(bbi[:], bim[:], crb, ALU.mult, ["b_im", "cr"], ["bbi"])
            tt(tq[:], bre[:], cib, ALU.mult, ["b_re", "ci", "bbr"], ["tq"])
            tt(bbi[:], bbi[:], tq[:], ALU.add, ["bbi", "tq"], ["bbi"])
            E = P_("E", [128, 16, 2, 16])
            TB = [P_("TB%d" % i, [128, 4, 128], BF16) for i in range(2)]
            CM = [P_("CM%d" % i, [128, 16, 2, 16], BF16) for i in range(2)]
            bu = [ps("bu%d" % i, [128, 512], ctx=ph) for i in range(4)]
            for ri, (src, sn_) in enumerate(((bbr, "bbr"), (bbi, "bbi"))):
                for g_ in range(2):
                    V(lambda e: e.tensor_scalar(out=E[:, :, g_, :], in0=src[:], scalar1=mask2[:, g_:g_ + 1], scalar2=None, op0=ALU.mult),
                      r=[sn_, "mask2"], w=["E"])
                for cc in range(4):
                    PE(lambda e: e.transpose(bu[ri][:, cc * 128:(cc + 1) * 128],
                                             E[:, 4 * cc:4 * cc + 4, :, :].rearrange("p q g c -> p (q g c)"), ident[:]),
                       r=["E", "ident"], w=["bu%d" % ri])
                V(lambda e: e.tensor_copy(out=TB[ri][:].rearrange("p c s -> p (c s)"), in_=bu[ri][:]), r=["bu%d" % ri], w=["TB%d" % ri])
            for g_ in range(2):
                V(lambda e: e.tensor_scalar(out=CM[0][:, :, g_, :], in0=cre[:], scalar1=mask2[:, g_:g_ + 1], scalar2=None, op0=ALU.mult),
                  r=["c_re", "mask2"], w=["CM0"])
                V(lambda e: e.tensor_scalar(out=CM[1][:, :, g_, :], in0=cim[:], scalar1=mask2[:, g_:g_ + 1], scalar2=-1.0, op0=ALU.mult, op1=ALU.mult),
                  r=["c_im", "mask2"], w=["CM1"])
            COST, SINT = P_("COST", [128, 16, 512]), P_("SINT", [128, 16, 512])
            for q in range(16):
                V(lambda e: e.tensor_scalar(out=SINT[:, q, :], in0=jidx[:], scalar1=sm["th"][:, q:q + 1], scalar2=None, op0=ALU.mult),
                  r=["th", "jidx"], w=["SINT"])
                sincos(SINT[:, q, :], ["SINT"], SINT[:, q, :], COST[:, q, :], 512, ["SINT"], ["COST"])
            V(lambda e: e.tensor_scalar(out=sm["u1"][:], in0=sm["th"][:], scalar1=512.0, scalar2=None, op0=ALU.mult), r=["th"], w=["u1"])
            sincos(sm["u1"][:], ["u1"], sm["s512"][:], sm["c512"][:], 16, ["s512"], ["c512"])
            V(lambda e: e.memset(sm["car"][:], 0.0), w=["car"])
            V(lambda e: e.memset(sm["cai"][:], 0.0), w=["cai"])

            uT = [P_("uT%d" % i, [128, 4, 512], BF16) for i in range(2)]
            S.barrier()
            wo = [{n_: P_("%s%d" % (n_, i), [128, 512]) for n_ in ("oa", "ob")} for i in range(2)]
            hb = [{n_: P_("%s%d" % (n_, i), [128, 512], BF16) for n_ in ("hre", "him")} for i in range(2)]
            ypre, sq_, inn = P_("ypre", [128, 512]), P_("sq_", [128, 512]), P_("inn", [128, 512])
            yg, ygb = P_("yg", [128, 4, 512]), P_("ygb", [128, 4, 512], BF16)
            s_, sqb = P_("s_", [128, 4, 512]), P_("sqb", [128, 4, 512], BF16)
            rs = P_("rs", [128, 512])
            yps = [ps("yps%d" % i, [128, 512], ctx=ph) for i in range(2)]
            zps = [ps("zps%d" % i, [128, 512], ctx=ph) for i in range(2)]
            UTv = R["UT"].rearrange("(cc p) t -> p cc t", p=128)
            zt = P_("zt", [128, 2048], BF16)
            V(lambda e: e.memset(zt[:], 0.0), w=["zt"])
            xsz = R["XSs"].rearrange("(n p r) f -> n p (r f)", p=128, r=2)
            nz_st = NROWS // 256 // 16
            for st in range(16 if lim >= 16 else min(lim, 16)):
                u_ = uT[st % 2]
                un = "uT%d" % (st % 2)
                if st == 0:
                    DMA(lambda e: e.dma_start(out=u_[:], in_=UTv[:, :, 0:512]), w=[un])
                if st + 1 < 16:
                    DMA(lambda e: e.dma_start(out=uT[(st + 1) % 2][:], in_=UTv[:, :, (st + 1) * 512:(st + 2) * 512]), w=["uT%d" % ((st + 1) % 2)])
                own = st >= 8
                pend = []
                for zi in range(nz_st * st, nz_st * (st + 1)):
                    DMA(lambda e: e.dma_start(out=xsz[zi], in_=zt[:]), r=["zt"], w=["XSs"])
                def fin_chunk(cc):
                    yp = yps[cc % 2]
                    ypn = "yps%d" % (cc % 2)
                    V(lambda e: e.scalar_tensor_tensor(out=ypre[:], in0=u_[:, cc, :], scalar=d_l[:, cc:cc + 1], in1=yp[:],
                                                       op0=ALU.mult, op1=ALU.add), r=[un, "d_l", ypn], w=["ypre"])
                    A(lambda e: e.activation(out=sq_[:], in_=ypre[:], func=AF.Square), r=["ypre"], w=["sq_"])
                    V(lambda e: e.tensor_scalar(out=inn[:], in0=sq_[:], scalar1=0.044715, scalar2=1.0, op0=ALU.mult, op1=ALU.add), r=["sq_"], w=["inn"])
                    tt(inn[:], inn[:], ypre[:], ALU.mult, ["inn", "ypre"], ["inn"])
                    A(lambda e: e.activation(out=sq_[:], in_=inn[:], func=AF.Sigmoid, scale=2.0 * math.sqrt(2.0 / PI)), r=["inn"], w=["sq_"])
                    tt(yg[:, cc, :], ypre[:], sq_[:], ALU.mult, ["ypre", "sq_"], ["yg%d" % cc])
                    A(lambda e: e.activation(out=ygb[:, cc, :], in_=yg[:, cc, :], func=AF.Copy), r=["yg%d" % cc], w=["ygb%d" % cc])

                def emit_bu(q_):
                    cc_, ql_, k_ = q_ // 4, q_ % 4, q_ % 2
                    PE(lambda e: e.matmul(bu[2 * k_][:], lhsT=TB[0][32 * ql_:32 * ql_ + 32, cc_, :], rhs=u_[32 * ql_:32 * ql_ + 32, cc_, :],
                                          start=True, stop=True, tile_position=(32 * ql_, 0)), r=["TB0", un], w=["bu%d" % (2 * k_)])
                    PE(lambda e: e.matmul(bu[2 * k_ + 1][:], lhsT=TB[1][32 * ql_:32 * ql_ + 32, cc_, :], rhs=u_[32 * ql_:32 * ql_ + 32, cc_, :],
                                          start=True, stop=True, tile_position=(32 * ql_, 0)), r=["TB1", un], w=["bu%d" % (2 * k_ + 1)])

                for q in range(16):
                    cc, ql = q // 4, q % 4
                    k = q % 2
                    W = wk[k]
                    wn = lambda n_: "%s%d" % (n_, k)
                    pr, pi2 = bu[2 * k], bu[2 * k + 1]
                    prn, pin = "bu%d" % (2 * k), "bu%d" % (2 * k + 1)
                    if q == 0:
                        emit_bu(0)
                    C_, S_ = COST[:, q, :], SINT[:, q, :]
                    tt(W["ta"][:], pr[:], C_, ALU.mult, [prn, "COST"], [wn("ta")])
                    tt(W["tb"][:], pi2[:], S_, ALU.mult, [pin, "SINT"], [wn("tb")])
                    tt(W["wre"][:], W["ta"][:], W["tb"][:], ALU.add, [wn("ta"), wn("tb")], [wn("wre")])
                    tt(W["ta"][:], pi2[:], C_, ALU.mult, [pin, "COST", wn("wre")], [wn("ta")])
                    tt(W["tb"][:], pr[:], S_, ALU.mult, [prn, "SINT", wn("wre")], [wn("tb")])
                    tt(W["wim"][:], W["ta"][:], W["tb"][:], ALU.subtract, [wn("ta"), wn("tb")], [wn("wim")])
                    rb = sm["rho"][:, q:q + 1].to_broadcast([128, 512])
                    V(lambda e: e.tensor_tensor_scan(out=W["gre"][:], data0=rb, data1=W["wre"][:], initial=sm["car"][:, q:q + 1],
                                                     op0=ALU.mult, op1=ALU.add), r=["rho", wn("wre"), "car"], w=[wn("gre")])
                    V(lambda e: e.tensor_tensor_scan(out=W["gim"][:], data0=rb, data1=W["wim"][:], initial=sm["cai"][:, q:q + 1],
                                                     op0=ALU.mult, op1=ALU.add), r=["rho", wn("wim"), "cai"], w=[wn("gim")])
                    A(lambda e: e.activation(out=sm["glr"][:, q:q + 1], in_=W["gre"][:, 511:512], func=AF.Copy), r=[wn("gre")], w=["glr"])
                    A(lambda e: e.activation(out=sm["gli"][:, q:q + 1], in_=W["gim"][:, 511:512], func=AF.Copy), r=[wn("gim")], w=["gli"])
                    if q + 1 < 16:
                        emit_bu(q + 1)
                    if own:
                        O, H = wo[k], hb[k]
                        on = lambda n_: "%s%d" % (n_, k)
                        gt = lambda o, a, b, op, r, w: G(lambda e: e.tensor_tensor(out=o, in0=a, in1=b, op=op), r=r, w=w)
                        tt(O["oa"][:], W["gre"][:], C_, ALU.mult, [wn("gre"), "COST"], [on("oa")])
                        gt(O["ob"][:], W["gim"][:], S_, ALU.mult, [wn("gim"), "SINT"], [on("ob")])
                        gt(H["hre"][:], O["oa"][:], O["ob"][:], ALU.subtract, [on("oa"), on("ob")], [on("hre")])
                        gt(O["oa"][:], W["gre"][:], S_, ALU.mult, [wn("gre"), "SINT", on("hre")], [on("oa")])
                        gt(O["ob"][:], W["gim"][:], C_, ALU.mult, [wn("gim"), "COST", on("hre")], [on("ob")])
                        gt(H["him"][:], O["oa"][:], O["ob"][:], ALU.add, [on("oa"), on("ob")], [on("him")])
                        yp = yps[cc % 2]
                        ypn = "yps%d" % (cc % 2)
                        PE(lambda e: e.matmul(yp[32 * ql:32 * ql + 32, :], lhsT=CM[0][:, q, :, :].rearrange("p g c -> p (g c)"), rhs=H["hre"][:],
                                              start=True, stop=False, tile_position=(0, 32 * ql)), r=["CM0", on("hre")], w=[ypn], sig=False)
                        PE(lambda e: e.matmul(yp[32 * ql:32 * ql + 32, :], lhsT=CM[1][:, q, :, :].rearrange("p g c -> p (g c)"), rhs=H["him"][:],
                                              start=False, stop=True, tile_position=(0, 32 * ql)), r=["CM1", on("him")], w=[ypn])
                        if ql == 3:
                            pend.append(cc)
                    if ql == 1 and pend:
                        fin_chunk(pend.pop(0))
                tt(sm["t1"][:], sm["glr"][:], sm["c512"][:], ALU.mult, ["glr", "c512"], ["t1"])
                tt(sm["t2"][:], sm["gli"][:], sm["s512"][:], ALU.mult, ["gli", "s512"], ["t2"])
                tt(sm["car"][:], sm["t1"][:], sm["t2"][:], ALU.subtract, ["t1", "t2"], ["car"])
                tt(sm["t1"][:], sm["glr"][:], sm["s512"][:], ALU.mult, ["glr", "s512", "car"], ["t1"])
                tt(sm["t2"][:], sm["gli"][:], sm["c512"][:], ALU.mult, ["gli", "c512", "car"], ["t2"])
                tt(sm["cai"][:], sm["t1"][:], sm["t2"][:], ALU.add, ["t1", "t2"], ["cai"])
                while pend:
                    fin_chunk(pend.pop(0))
                if st == 7:
                    V(lambda e: e.tensor_scalar(out=sm["car"][:], in0=sm["car"][:], scalar1=flag[:, 0:1], scalar2=None, op0=ALU.mult), r=["car", "flag"], w=["car"])
                    V(lambda e: e.tensor_scalar(out=sm["cai"][:], in0=sm["cai"][:], scalar1=flag[:, 0:1], scalar2=None, op0=ALU.mult), r=["cai", "flag"], w=["cai"])
                if own:
                    t0 = (st - 8) * 512
                    ygr = ["ygb%d" % c_ for c_ in range(4)]
                    for oc in range(4):
                        zp = zps[oc % 2]
                        zn = "zps%d" % (oc % 2)
                        for cc in range(4):
                            PE(lambda e: e.matmul(zp[:], lhsT=wglub[:, cc, oc * 128:(oc + 1) * 128], rhs=ygb[:, cc, :], start=(cc == 0), stop=(cc == 3)),
                               r=["wglub"] + ygr, w=[zn], sig=(cc == 3))
                        A(lambda e: e.activation(out=rs[:], in_=zp[:], func=AF.Sigmoid, bias=bglu[:, oc:oc + 1]), r=[zn, "bglu_l"], w=["rs"])
                        tt(s_[:, oc, :], yg[:, oc, :], rs[:], ALU.mult, ["yg%d" % oc, "rs"], ["s_%d" % oc])
                        A(lambda e: e.activation(out=sqb[:, oc, :], in_=s_[:, oc, :], func=AF.Square), r=["s_%d" % oc], w=["sqb%d" % oc])
                    zp = zps[0]
                    for oc in range(4):
                        PE(lambda e: e.matmul(zp[:], lhsT=onesb[:], rhs=sqb[:, oc, :], start=(oc == 0), stop=(oc == 3)),
                           r=["onesb"] + ["sqb%d" % o_ for o_ in range(4)], w=["zps0"], sig=(oc == 3))
                    V(lambda e: e.tensor_scalar(out=rs[:], in0=zp[:], scalar1=1.0 / 512, scalar2=EPS, op0=ALU.mult, op1=ALU.add), r=["zps0"], w=["rs"])
                    A(lambda e: e.activation(out=rs[:], in_=rs[:], func=AF.Sqrt), r=["rs"], w=["rs"])
                    V(lambda e: e.reciprocal(out=rs[:], in_=rs[:]), r=["rs"], w=["rs"])
                    for oc in range(4):
                        V(lambda e: e.scalar_tensor_tensor(out=ssmn[:, oc, t0:t0 + 512], in0=s_[:, oc, :], scalar=gssm[:, oc:oc + 1], in1=rs[:],
                                                           op0=ALU.mult, op1=ALU.mult), r=["s_%d" % oc, "gssm_l", "rs"], w=["ssmn"])
            if "ssm" in dbg:
                dd = dbg_dram("ssmn", [512, OWN], BF16)
                DMA(lambda e: e.dma_start(out=dd.rearrange("(oc p) t -> p oc t", p=128), in_=ssmn[:]), r=["ssmn"], w=["dbgssm"])
            S.barrier()
        if stage == 2:
            return finish(nc, S, out, dbg_out, I)


        ph23 = top.enter_context(ExitStack())
        attT = sb("attT", [128, 4, OWN], BF16, ctx=ph23)
        with ExitStack() as ph:
            P_ = lambda name, shape, dt=F32: sb(name, shape, dt, ctx=ph)
            tbf = P_("tbf", [128, 24, 2, 128])
            mkt = P_("mkt", [128, 2, 128])
            biasb = P_("biasb", [128, 24, 2, 128], BF16)
            prev0b = P_("prev0b", [128, 24, 128], BF16)
            pm = P_("pm", [128, 1])
            ones64 = P_("ones64", [128, 64], BF16)
            qTc = P_("qTc", [128, OWN], BF16)
            kTc = P_("kTc", [128, 6144], BF16)
            Vd = [P_("Vd%d" % i, [128, 48, 128], BF16) for i in range(3)]
            acc = P_("acc", [128, 2, OWN])
            pTs = [P_("pTs%d" % i, [128, 2, 2, 128], BF16) for i in range(2)]
            sp = [ps("sp%d" % i, [128, 2, 2, 128], ctx=ph) for i in range(2)]
            opp = [ps("opp%d" % i, [128, 4, 128], ctx=ph) for i in range(2)]
            DMA(lambda e: e.dma_start(out=tbf[:].rearrange("p a k j -> p (a k) j"), in_=I["tb"]), w=["tbf"])
            DMA(lambda e: e.dma_start(out=mkt[:], in_=I["mk"]), w=["mkt"])
            V(lambda e: e.memset(ones64[:], 1.0), w=["ones64"])
            V(lambda e: e.tensor_scalar(out=pm[:], in0=flag[:], scalar1=-1.0, scalar2=-NEGM, op0=ALU.add, op1=ALU.mult), r=["flag"], w=["pm"])
            for kt in range(2):
                V(lambda e: e.tensor_tensor(out=biasb[:, :, kt, :], in0=tbf[:, :, kt, :], in1=mkt[:, kt, :].unsqueeze(1).to_broadcast([128, 24, 128]),
                                            op=ALU.add), r=["tbf", "mkt"], w=["biasb"])
            V(lambda e: e.tensor_scalar(out=prev0b[:], in0=biasb[:, :, 0, :], scalar1=pm[:, 0:1], scalar2=None, op0=ALU.add), r=["biasb", "pm"], w=["prev0b"])
            for c in range(alim):
                DMA(lambda e: e.dma_start(out=qTc[:], in_=R["QT"][c * 128:(c + 1) * 128, :]), w=["qTc"])
                DMA(lambda e: e.dma_start(out=kTc[:], in_=R["KT"][c * 128:(c + 1) * 128, :]), w=["kTc"])
                for pi, d in enumerate(DILS):
                    vv = R["VS"].rearrange("(m i d) f -> d i m f", i=128, d=d)
                    nt = 48 // d
                    for r in range(d):
                        DMA(lambda e: e.dma_start(out=Vd[pi][:, r * nt:(r + 1) * nt, :], in_=vv[r][:, :, c * 128:(c + 1) * 128]), w=["Vd%d" % pi])
                units = [(pi, d, r, n) for pi, d in enumerate(DILS) for r in range(d) for n in range(32 // d)]

                def stS(ui):
                    pi, d, r, n = units[ui]
                    m_cur = 16 // d + n
                    b = ui % 2
                    sp_, pT_ = sp[b], pTs[b]
                    q0 = r + 128 * n * d
                    qsl = slice(q0, q0 + 127 * d + 1, d)
                    for hh in range(2):
                        hs = slice(64 * hh, 64 * hh + 64)
                        for kt, m in ((0, m_cur - 1), (1, m_cur)):
                            k0 = r + 128 * m * d
                            PE(lambda e: e.matmul(sp_[:, hh, kt, :], lhsT=kTc[hs, k0:k0 + 127 * d + 1:d], rhs=qTc[hs, qsl],
                                                  start=True, stop=False, tile_position=(64 * hh, 0)), r=["kTc", "qTc"], w=["sp%d" % b], sig=False)
                            ph_ = pi * 8 + 2 * c + hh
                            bt = prev0b[:, ph_, :] if (kt == 0 and n == 0) else biasb[:, ph_, kt, :]
                            PE(lambda e: e.matmul(sp_[:, hh, kt, :], lhsT=identb[:], rhs=bt, start=False, stop=True),
                               r=["identb", "biasb", "prev0b"], w=["sp%d" % b], sig=(hh == 1 and kt == 1))
                    A(lambda e: e.activation(out=pT_[:].rearrange("p a b j -> p (a b j)"), in_=sp_[:].rearrange("p a b j -> p (a b j)"), func=AF.Exp),
                      r=["sp%d" % b], w=["pTs%d" % b])

                def stPV(ui):
                    pi, d, r, n = units[ui]
                    nt = 48 // d
                    m_cur = 16 // d + n
                    b = ui % 2
                    op_, pT_ = opp[b], pTs[b]
                    q0 = r + 128 * n * d
                    qsl = slice(q0, q0 + 127 * d + 1, d)
                    for hh in range(2):
                        hs = slice(64 * hh, 64 * hh + 64)
                        for kt, m in ((0, m_cur - 1), (1, m_cur)):
                            PE(lambda e: e.matmul(op_[hs, 0, :], lhsT=Vd[pi][:, r * nt + m, hs], rhs=pT_[:, hh, kt, :],
                                                  start=(kt == 0), stop=(kt == 1), tile_position=(0, 64 * hh)),
                               r=["Vd%d" % pi, "pTs%d" % b], w=["opp%d" % b], sig=False)
                        for kt in range(2):
                            PE(lambda e: e.matmul(op_[hs, 1, :], lhsT=ones64[:], rhs=pT_[:, hh, kt, :],
                                                  start=(kt == 0), stop=(kt == 1), tile_position=(0, 64 * hh)),
                               r=["ones64", "pTs%d" % b], w=["opp%d" % b], sig=(hh == 1 and kt == 1))
                    if pi == 0:
                        V(lambda e: e.tensor_copy(out=acc[:, :, qsl], in_=op_[:, 0:2, :]), r=["opp%d" % b], w=["acc"])
                    else:
                        V(lambda e: e.tensor_tensor(out=acc[:, :, qsl], in0=op_[:, 0:2, :], in1=acc[:, :, qsl], op=ALU.add), r=["opp%d" % b, "acc"], w=["acc"])

                stS(0)
                for ui in range(len(units)):
                    if ui + 1 < len(units):
                        stS(ui + 1)
                    stPV(ui)
                V(lambda e: e.reciprocal(out=acc[:, 1, :], in_=acc[:, 1, :]), r=["acc"], w=["acc"])
                V(lambda e: e.tensor_tensor(out=attT[:, c, :], in0=acc[:, 0, :], in1=acc[:, 1, :], op=ALU.mult), r=["acc"], w=["attT"])
            if "att" in dbg:
                dd = dbg_dram("attT", [512, OWN], BF16)
                DMA(lambda e: e.dma_start(out=dd.rearrange("(oc p) t -> p oc t", p=128)[:, 0:alim, :], in_=attT[:, 0:alim, :]), r=["attT"], w=["dbgatt"])
            S.barrier()
        if stage == 3:
            return finish(nc, S, out, dbg_out, I)


        def bcast_row(dst, src_row, name):
            DMA(lambda e: e.dma_start(out=dst[:], in_=src_row.to_broadcast([128, src_row.shape[1]])), r=["ADA"], w=[name])

        def layer_norm(xr, xres, outt, outres, g_t, b_t, gname, bname, st6, mv, rstd):
            for hf in range(2):
                V(lambda e: e.bn_stats(out=st6[:, hf, :], in_=xr[:, hf * 512:(hf + 1) * 512]), r=[xres], w=["st6"])
            V(lambda e: e.bn_aggr(out=mv[:], in_=st6[:].rearrange("p a b -> p (a b)")), r=["st6"], w=["mv"])
            V(lambda e: e.tensor_scalar_add(out=rstd[:], in0=mv[:, 1:2], scalar1=EPS), r=["mv"], w=["rstd"])
            A(lambda e: e.activation(out=rstd[:], in_=rstd[:], func=AF.Sqrt), r=["rstd"], w=["rstd"])
            V(lambda e: e.reciprocal(out=rstd[:], in_=rstd[:]), r=["rstd"], w=["rstd"])
            V(lambda e: e.tensor_scalar(out=outt[:], in0=xr[:], scalar1=mv[:, 0:1], scalar2=rstd[:, 0:1], op0=ALU.subtract, op1=ALU.mult),
              r=[xres, "mv", "rstd"], w=[outres])
            V(lambda e: e.tensor_tensor(out=outt[:], in0=outt[:], in1=g_t[:], op=ALU.mult), r=[outres, gname], w=[outres])
            V(lambda e: e.tensor_tensor(out=outt[:], in0=outt[:], in1=b_t[:], op=ALU.add), r=[outres, bname], w=[outres])

        with ExitStack() as ph:
            P_ = lambda name, shape, dt=F32: sb(name, shape, dt, ctx=ph)
            woutg = P_("woutg", [128, 8, D], BF16)
            wrb = P_("wrb", [128, 8, NEXP], BF16)
            wsg, wsu = P_("wsg", [128, 8, 256], BF16), P_("wsu", [128, 8, 256], BF16)
            wsd = P_("wsd", [128, 2, D], BF16)
            g1bc, ln1g, ln1b, sc2, sh2 = (P_(n_, [128, D]) for n_ in ("g1bc", "ln1g", "ln1b", "sc2", "sh2"))
            rbbc, ebase, eidx1, cntbc = (P_(n_, [128, NEXP]) for n_ in ("rbbc", "ebase", "eidx1", "cntbc"))
            gatt = P_("gatt", [128, 4])
            wst = [P_("wst0", [128, D])] * 2
            bcast_row(g1bc, R["ADA"][0:1, :], "g1bc")
            bcast_row(sh2, R["ADA"][1:2, :], "sh2")
            bcast_row(sc2, R["ADA"][2:3, :], "sc2")
            bcast_row(ln1g, I["ln1"][0:1, :], "ln1g")
            bcast_row(ln1b, I["ln1"][1:2, :], "ln1b")
            bcast_row(rbbc, I["rbias"][0:1, :], "rbbc")
            for t_, k_ in ((ebase, "ebase"), (eidx1, "eidx1"), (gatt, "gatt_l")):
                DMA(lambda e: e.dma_start(out=t_[:], in_=I[k_]), w=[k_])
            V(lambda e: e.memset(cntbc[:], 0.0), w=["cntbc"])
            DMA(lambda e: e.dma_start(out=wrb[:], in_=I["w_router"].rearrange("(kc p) n -> p kc n", p=128)), w=["wrb"], q="pool")
            DMA(lambda e: e.dma_start(out=wsg[:], in_=I["w_sg"].rearrange("(kc p) n -> p kc n", p=128)), w=["wsg"], q="pool")
            DMA(lambda e: e.dma_start(out=wsu[:], in_=I["w_su"].rearrange("(kc p) n -> p kc n", p=128)), w=["wsu"], q="pool")
            DMA(lambda e: e.dma_start(out=wsd[:], in_=I["w_sd"].rearrange("(fc p) n -> p fc n", p=128)), w=["wsd"], q="pool")
            wov = I["w_out"].rearrange("(kc p) n -> p kc n", p=128)
            for kc in range(8):
                DMA(lambda e: e.dma_start(out=wst[0][:], in_=wov[:, kc, :]), w=["wst0"])
                V(lambda e: e.tensor_tensor(out=woutg[:, kc, :], in0=wst[0][:], in1=g1bc[:], op=ALU.mult), r=["wst0", "g1bc"], w=["woutg"])
            sqa = P_("sqa", [128, 4, 512], BF16)
            matts = [P_("matt%d" % i, [128, 4, 512], BF16) for i in range(2)]
            rsa = P_("rsa", [128, 512])
            xt3 = [P_("x3t%d" % i, [128, D]) for i in range(2)]
            xr = P_("xr", [128, D])
            x1t = [P_("x1t%d" % i, [128, D]) for i in range(2)]
            h2f = P_("h2f", [128, D])
            h2b = [P_("h2b%d" % i, [128, D], BF16) for i in range(2)]
            h2Ts = [P_("h2T%d" % i, [128, 8, 128], BF16) for i in range(2)]
            st6, mv, rstd = P_("st6", [128, 2, 6]), P_("mv", [128, 2]), P_("rstd", [128, 1])
            rt = {n_: P_("r_" + n_, [128, NEXP]) for n_ in ("s", "biased", "masked", "sel", "wsel", "Gd", "destf", "junk", "eidsel")}
            selb = P_("selb", [128, NEXP], BF16)
            m8, gs, gsort, gok, pen = P_("m8", [128, 8, 8]), P_("gs", [128, 8]), P_("gsort", [128, 8]), P_("gok", [128, 8]), P_("pen", [128, 8])
            t8, v8, dk, dsum = P_("t8", [128, 8]), P_("v8", [128, 8]), P_("dk", [128, 8]), P_("dsum", [128, 1])
            hidT, sgs = P_("hidT", [128, 2, 128], BF16), P_("sgs", [128, 2, 128])
            shs = [P_("shs%d" % i, [128, D]) for i in range(2)]
            mixps = ps("mixps", [128, D], ctx=ph)
            shps = ps("shps", [128, D], ctx=ph)
            h2Tps = ps("h2Tps", [128, 8, 128], BF16, ctx=ph)
            rps = ps("rps", [128, 512], ctx=ph)
            cumps = ps("cumps", [128, 512], ctx=ph)
            hsps = ps("hsps", [128, 4, 128], ctx=ph)
            def grp(gp):
                g0 = gp * 512
                matt = matts[gp % 2]
                mn = "matt%d" % (gp % 2)
                for c in range(4):
                    A(lambda e: e.activation(out=sqa[:, c, :], in_=attT[:, c, g0:g0 + 512], func=AF.Square), r=["attT"], w=["sqa"])
                for c in range(4):
                    PE(lambda e: e.matmul(hsps[:].rearrange("p a b -> p (a b)"), lhsT=onesb[:], rhs=sqa[:, c, :], start=(c == 0), stop=(c == 3)),
                       r=["onesb", "sqa"], w=["hsps"], sig=(c == 3))
                V(lambda e: e.tensor_scalar(out=rsa[:], in0=hsps[:].rearrange("p a b -> p (a b)"), scalar1=1.0 / 512, scalar2=EPS, op0=ALU.mult, op1=ALU.add),
                  r=["hsps"], w=["rsa"])
                A(lambda e: e.activation(out=rsa[:], in_=rsa[:], func=AF.Sqrt), r=["rsa"], w=["rsa"])
                V(lambda e: e.reciprocal(out=rsa[:], in_=rsa[:]), r=["rsa"], w=["rsa"])
                for c in range(4):
                    V(lambda e: e.scalar_tensor_tensor(out=matt[:, c, :], in0=attT[:, c, g0:g0 + 512], scalar=gatt[:, c:c + 1], in1=rsa[:],
                                                       op0=ALU.mult, op1=ALU.mult), r=["attT", "gatt_l", "rsa"], w=[mn])

            def S1(ti):
                gp, sub = ti // 4, ti % 4
                matt = matts[gp % 2]
                mn = "matt%d" % (gp % 2)
                h2T = h2Ts[ti % 2]
                h2Tn = "h2T%d" % (ti % 2)
                tok0 = ti * 128
                k2 = ti % 2
                x_, x1_, hb_, sh_ = xt3[k2], x1t[k2], h2b[k2], shs[k2]
                xn_, x1n, hbn, shn = "x3t%d" % k2, "x1t%d" % k2, "h2b%d" % k2, "shs%d" % k2
                DMA(lambda e: e.dma_start(out=x_[:], in_=I["xs"][OWN + tok0:OWN + tok0 + 128, :]), w=[xn_])
                for nh in range(2):
                    for kc in range(8):
                        lh = matt[:, kc, sub * 128:(sub + 1) * 128] if kc < 4 else ssmn[:, kc - 4, tok0:tok0 + 128]
                        PE(lambda e: e.matmul(mixps[:, nh * 512:(nh + 1) * 512], lhsT=lh, rhs=woutg[:, kc, nh * 512:(nh + 1) * 512],
                                              start=(kc == 0), stop=(kc == 7)), r=[mn, "ssmn", "woutg"], w=["mixps"], sig=(kc == 7 and nh == 1))
                V(lambda e: e.scalar_tensor_tensor(out=xr[:], in0=x_[:], scalar=ALPHA, in1=mixps[:], op0=ALU.mult, op1=ALU.add),
                  r=[xn_, "mixps"], w=["xr"])
                layer_norm(xr, "xr", x1_, x1n, ln1g, ln1b, "ln1g", "ln1b", st6, mv, rstd)
                DMA(lambda e: e.dma_start(out=R["X1"][tok0:tok0 + 128, :], in_=x1_[:]), r=[x1n], w=["X1"], q="pool")
                V(lambda e: e.tensor_tensor(out=h2f[:], in0=x1_[:], in1=sc2[:], op=ALU.mult), r=[x1n, "sc2"], w=["h2f"])
                V(lambda e: e.tensor_tensor(out=hb_[:], in0=h2f[:], in1=sh2[:], op=ALU.add), r=["h2f", "sh2"], w=[hbn])
                for kc in range(8):
                    PE(lambda e: e.transpose(h2Tps[:, kc, :], hb_[:, kc * 128:(kc + 1) * 128], identb[:]), r=[hbn, "identb"], w=["h2Tps"])
                A(lambda e: e.activation(out=h2T[:].rearrange("p a b -> p (a b)"), in_=h2Tps[:].rearrange("p a b -> p (a b)"), func=AF.Copy),
                  r=["h2Tps"], w=[h2Tn])

            def S2(ti):
                tok0 = ti * 128
                k2 = ti % 2
                hb_, sh_ = h2b[k2], shs[k2]
                hbn, shn = "h2b%d" % k2, "shs%d" % k2
                h2T = h2Ts[ti % 2]
                h2Tn = "h2T%d" % (ti % 2)
                for kc in range(8):
                    PE(lambda e: e.matmul(rps[:, 0:NEXP], lhsT=h2T[:, kc, :], rhs=wrb[:, kc, :], start=(kc == 0), stop=(kc == 7)),
                       r=[h2Tn, "wrb"], w=["rps"], sig=(kc == 7))
                A(lambda e: e.activation(out=rt["s"][:], in_=rps[:, 0:NEXP], func=AF.Sigmoid), r=["rps"], w=["r_s"])
                V(lambda e: e.tensor_tensor(out=rt["biased"][:], in0=rt["s"][:], in1=rbbc[:], op=ALU.add), r=["r_s", "rbbc"], w=["r_biased"])
                for g_ in range(8):
                    V(lambda e: e.max(out=m8[:, g_, :], in_=rt["biased"][:, g_ * 32:(g_ + 1) * 32]), r=["r_biased"], w=["m8"])
                V(lambda e: e.tensor_tensor(out=gs[:], in0=m8[:, :, 0], in1=m8[:, :, 1], op=ALU.add), r=["m8"], w=["gs"])
                V(lambda e: e.max(out=gsort[:], in_=gs[:]), r=["gs"], w=["gsort"])
                V(lambda e: e.tensor_scalar(out=gok[:], in0=gs[:], scalar1=gsort[:, 3:4], scalar2=None, op0=ALU.is_ge), r=["gs", "gsort"], w=["gok"])
                V(lambda e: e.tensor_scalar(out=pen[:], in0=gok[:], scalar1=-1.0, scalar2=1e9, op0=ALU.add, op1=ALU.mult), r=["gok"], w=["pen"])
                V(lambda e: e.tensor_tensor(out=rt["masked"][:].rearrange("p (g e) -> p g e", g=8), in0=rt["biased"][:].rearrange("p (g e) -> p g e", g=8),
                                            in1=pen[:].unsqueeze(2).to_broadcast([128, 8, 32]), op=ALU.add), r=["r_biased", "pen"], w=["r_masked"])
                V(lambda e: e.max(out=t8[:], in_=rt["masked"][:]), r=["r_masked"], w=["t8"])
                V(lambda e: e.tensor_scalar(out=rt["sel"][:], in0=rt["masked"][:], scalar1=t8[:, 7:8], scalar2=None, op0=ALU.is_ge), r=["r_masked", "t8"], w=["r_sel"])
                V(lambda e: e.tensor_tensor(out=rt["wsel"][:], in0=rt["sel"][:], in1=rt["s"][:], op=ALU.mult), r=["r_sel", "r_s"], w=["r_wsel"])
                V(lambda e: e.reduce_sum(out=dsum[:], in_=rt["wsel"][:], axis=mybir.AxisListType.X), r=["r_wsel"], w=["dsum"])
                V(lambda e: e.reciprocal(out=dsum[:], in_=dsum[:]), r=["dsum"], w=["dsum"])
                V(lambda e: e.tensor_scalar(out=rt["Gd"][:], in0=rt["wsel"][:], scalar1=dsum[:, 0:1], scalar2=2.5, op0=ALU.mult, op1=ALU.mult), r=["r_wsel", "dsum"], w=["r_Gd"])
                A(lambda e: e.activation(out=selb[:], in_=rt["sel"][:], func=AF.Copy), r=["r_sel"], w=["selb"])
                PE(lambda e: e.matmul(cumps[:, 0:NEXP], lhsT=ltrib[:], rhs=selb[:], start=True, stop=True), r=["ltrib", "selb"], w=["cumps"], sig=False)
                PE(lambda e: e.matmul(cumps[:, NEXP:2 * NEXP], lhsT=onesb[:], rhs=selb[:], start=True, stop=True), r=["onesb", "selb"], w=["cumps"])
                V(lambda e: e.tensor_tensor(out=rt["destf"][:], in0=cumps[:, 0:NEXP], in1=cntbc[:], op=ALU.add), r=["cumps", "cntbc"], w=["r_destf"])
                V(lambda e: e.tensor_tensor(out=cntbc[:], in0=cumps[:, NEXP:2 * NEXP], in1=cntbc[:], op=ALU.add), r=["cumps", "cntbc", "r_destf"], w=["cntbc"])
                V(lambda e: e.tensor_tensor(out=rt["eidsel"][:], in0=rt["sel"][:], in1=eidx1[:], op=ALU.mult), r=["r_sel", "eidx1"], w=["r_eidsel"])
                V(lambda e: e.max(out=v8[:], in_=rt["eidsel"][:]), r=["r_eidsel"], w=["v8"])
                for k in range(8):
                    V(lambda e: e.scalar_tensor_tensor(out=rt["junk"][:], in0=rt["eidsel"][:], scalar=v8[:, k:k + 1], in1=rt["destf"][:],
                                                       op0=ALU.is_equal, op1=ALU.mult, accum_out=dk[:, k:k + 1]), r=["r_eidsel", "v8", "r_destf"], w=["r_junk", "dk"])
                    V(lambda e: e.scalar_tensor_tensor(out=rt["junk"][:], in0=rt["eidsel"][:], scalar=v8[:, k:k + 1], in1=rt["Gd"][:],
                                                       op0=ALU.is_equal, op1=ALU.mult, accum_out=GATE[:, ti * 8 + k:ti * 8 + k + 1]),
                      r=["r_eidsel", "v8", "r_Gd"], w=["r_junk", "GATE"])
                V(lambda e: e.tensor_copy(out=POS[:, ti * 8:(ti + 1) * 8], in_=dk[:]), r=["dk"], w=["POS"])
                V(lambda e: e.tensor_copy(out=V8S[:, ti * 8:(ti + 1) * 8], in_=v8[:]), r=["v8"], w=["V8S"])
                DMA(lambda e: e.dma_start(out=R["H2"][tok0:tok0 + 128, :], in_=hb_[:]), r=[hbn], w=["H2"], q="pool")
                for fc in range(2):
                    for gu, wt_, wtn in ((0, wsg, "wsg"), (1, wsu, "wsu")):
                        for kc in range(8):
                            PE(lambda e: e.matmul(hsps[:, fc * 2 + gu, :], lhsT=wt_[:, kc, fc * 128:(fc + 1) * 128], rhs=h2T[:, kc, :],
                                                  start=(kc == 0), stop=(kc == 7)), r=[wtn, h2Tn], w=["hsps"], sig=(kc == 7 and fc == 1 and gu == 1))
                for fc in range(2):
                    A(lambda e: e.activation(out=sgs[:, fc, :], in_=hsps[:, fc * 2, :], func=AF.Silu), r=["hsps"], w=["sgs"])
                    V(lambda e: e.tensor_tensor(out=hidT[:, fc, :], in0=hsps[:, fc * 2 + 1, :], in1=sgs[:, fc, :], op=ALU.mult), r=["hsps", "sgs"], w=["hidT"])
                for nh in range(2):
                    for fc in range(2):
                        PE(lambda e: e.matmul(shps[:, nh * 512:(nh + 1) * 512], lhsT=hidT[:, fc, :], rhs=wsd[:, fc, nh * 512:(nh + 1) * 512],
                                              start=(fc == 0), stop=(fc == 1)), r=["hidT", "wsd"], w=["shps"], sig=(fc == 1 and nh == 1))
                A(lambda e: e.activation(out=sh_[:], in_=shps[:], func=AF.Copy), r=["shps"], w=[shn])
                DMA(lambda e: e.dma_start(out=R["SHs"][tok0:tok0 + 128, :], in_=sh_[:]), r=[shn], w=["SHs"], q="pool")

            n_t3 = glim * 4
            grp(0)
            S1(0)
            for ti in range(n_t3):
                if ti + 1 < n_t3:
                    if (ti + 1) % 4 == 0:
                        grp((ti + 1) // 4)
                    S1(ti + 1)
                S2(ti)
            I32_ = mybir.dt.int32
            nbi = P_("nbi", [128, NEXP], I32_)
            nb, incl, bs128, onesf = P_("nb", [128, NEXP]), P_("incl", [128, NEXP]), P_("bs128", [128, NEXP]), P_("onesf", [128, NEXP])
            bendT, pidx, bidx = P_("bendT", [128, 2]), P_("pidx", [128, 1]), P_("bidx", [128, NBLK])
            cmpb = P_("cmpb", [128, 2, NBLK], BF16)
            ebf = P_("ebf", [128, NBLK])
            DMA(lambda e: e.dma_start(out=pidx[:], in_=I["pidx"]), w=["pidx"])
            DMA(lambda e: e.dma_start(out=bidx[:], in_=I["bidx"]), w=["bidx"])
            V(lambda e: e.memset(onesf[:], 1.0), w=["onesf"])
            V(lambda e: e.tensor_scalar(out=nbi[:], in0=cntbc[:], scalar1=1.0 / BR, scalar2=(BR - 1.0) / BR - 0.5 + 0.5 / BR, op0=ALU.mult, op1=ALU.add), r=["cntbc"], w=["nbi"])
            V(lambda e: e.tensor_copy(out=nb[:], in_=nbi[:]), r=["nbi"], w=["nb"])
            V(lambda e: e.tensor_tensor_scan(out=incl[:], data0=onesf[:], data1=nb[:], initial=0.0, op0=ALU.mult, op1=ALU.add), r=["onesf", "nb"], w=["incl"])
            V(lambda e: e.tensor_tensor(out=bs128[:], in0=incl[:], in1=nb[:], op=ALU.subtract), r=["incl", "nb"], w=["bs128"])
            V(lambda e: e.tensor_scalar(out=bs128[:], in0=bs128[:], scalar1=float(BR), scalar2=None, op0=ALU.mult), r=["bs128"], w=["bs128"])
            for c in range(2):
                PE(lambda e: e.transpose(rps[:, c * 128:(c + 1) * 128], incl[:, c * 128:(c + 1) * 128], ident[:]), r=["incl", "ident"], w=["rps"])
            V(lambda e: e.tensor_copy(out=bendT[:], in_=rps[:, 0:256:128]), r=["rps"], w=["bendT"])
            for c in range(2):
                V(lambda e: e.tensor_scalar(out=cmpb[:, c, :], in0=bidx[:], scalar1=bendT[:, c:c + 1], scalar2=None, op0=ALU.is_ge), r=["bidx", "bendT"], w=["cmpb"])
            for c in range(2):
                PE(lambda e: e.matmul(cumps[:, 0:NBLK], lhsT=onesb[:], rhs=cmpb[:, c, :], start=(c == 0), stop=(c == 1)), r=["onesb", "cmpb"], w=["cumps"], sig=(c == 1))
            V(lambda e: e.tensor_scalar(out=ebf[:], in0=cumps[:, 0:NBLK], scalar1=128.0, scalar2=None, op0=ALU.mult), r=["cumps"], w=["ebf"])
            V(lambda e: e.tensor_scalar(out=IDXW[:], in0=ebf[:], scalar1=pidx[:, 0:1], scalar2=None, op0=ALU.add), r=["ebf", "pidx"], w=["IDXW"])
            for ti in range(glim * 4):
                hb_, hbn = h2b[ti % 2], "h2b%d" % (ti % 2)
                DMA(lambda e: e.dma_start(out=hb_[:], in_=R["H2"][ti * 128:(ti + 1) * 128, :]), r=["H2"], w=[hbn])
                for k in range(8):
                    V(lambda e: e.scalar_tensor_tensor(out=rt["junk"][:], in0=eidx1[:], scalar=V8S[:, ti * 8 + k:ti * 8 + k + 1], in1=bs128[:],
                                                       op0=ALU.is_equal, op1=ALU.mult, accum_out=dk[:, k:k + 1]), r=["eidx1", "V8S", "bs128"], w=["r_junk", "dk"])
                V(lambda e: e.tensor_tensor(out=dk[:], in0=dk[:], in1=POS[:, ti * 8:(ti + 1) * 8], op=ALU.add), r=["dk", "POS"], w=["dk"])
                V(lambda e: e.tensor_copy(out=DEST[:, ti * 8:(ti + 1) * 8], in_=dk[:]), r=["dk"], w=["DEST%d" % ti])
                for k in range(8):
                    DMA(lambda e: e.indirect_dma_start(out=R["XSs"], out_offset=bass.IndirectOffsetOnAxis(ap=DEST[:, ti * 8 + k:ti * 8 + k + 1], axis=0),
                                                       in_=hb_[:], in_offset=None), r=[hbn, "DEST%d" % ti], w=["XSs_sc%d_%d" % (ti, k)], q="pool")
            if "x1" in dbg:
                dd6 = dbg_dram("dest", [128, 256], U32)
                DMA(lambda e: e.dma_start(out=dd6[:, 0:glim * 32], in_=DEST[:, 0:glim * 32]), r=["DEST%d" % t_ for t_ in range(glim * 4)], w=["dbgdest"])
                dd7 = dbg_dram("idxw", [128, NBLK], U32)
                DMA(lambda e: e.dma_start(out=dd7, in_=IDXW[:]), r=["IDXW"], w=["dbgidxw"])
                dd = dbg_dram("x1", [OWN, D])
                DMA(lambda e: e.dma_start(out=dd[0:glim * 512, :], in_=R["X1"][0:glim * 512, :]), r=["X1"], w=["dbgx1"])
                dd2 = dbg_dram("v8s", [128, 256])
                DMA(lambda e: e.dma_start(out=dd2[:, 0:glim * 32], in_=V8S[:, 0:glim * 32]), r=["V8S"], w=["dbgv8s"])
                dd3 = dbg_dram("gate", [128, 256])
                DMA(lambda e: e.dma_start(out=dd3[:, 0:glim * 32], in_=GATE[:, 0:glim * 32]), r=["GATE"], w=["dbggate"])
                dd4 = dbg_dram("shs", [OWN, D])
                DMA(lambda e: e.dma_start(out=dd4[0:glim * 512, :], in_=R["SHs"][0:glim * 512, :]), r=["SHs"], w=["dbgshs"])
                dd5 = dbg_dram("cnt", [128, NEXP])
                DMA(lambda e: e.dma_start(out=dd5, in_=cntbc[:]), r=["cntbc"], w=["dbgcnt"])
            S.barrier()
        ph23.close()
        if stage == 4:
            return finish(nc, S, out, dbg_out, I)


        with ExitStack() as ph:
            P_ = lambda name, shape, dt=F32: sb(name, shape, dt, ctx=ph)
            NW = 4
            wgb = [P_("wgb%d" % i, [128, 8, 256], BF16) for i in range(NW)]
            wub = [P_("wub%d" % i, [128, 8, 256], BF16) for i in range(NW)]
            wdb = [P_("wdb%d" % i, [128, 2, D], BF16) for i in range(NW)]
            NX = 3
            Xb = [P_("Xb%d" % i, [128, 2, D], BF16) for i in range(NX)]
            Yb = [P_("Yb%d" % i, [128, 2, D], BF16) for i in range(2)]
            xT = [P_("xT%d" % i, [128, 8, BR], BF16) for i in range(2)]
            sg4 = P_("sg4", [128, 2, BR])
            hT4 = [P_("hT4_%d" % i, [128, 2, BR], BF16) for i in range(2)]
            xTp = ps("xTp", [128, 8, BR], BF16, ctx=ph)
            hps = ps("hps", [128, 4, BR], ctx=ph)
            yps = [ps("yps4_%d" % i, [128, D], ctx=ph) for i in range(2)]
            wgv = I["w_eg"].rearrange("e (p kc) f -> (e p) (kc f)", kc=8)
            wuv = I["w_eu"].rearrange("e (p kc) f -> (e p) (kc f)", kc=8)
            wdv = I["w_ed"].rearrange("e (p fc) n -> (e p) (fc n)", fc=2)

            bc_reg = nc.gpsimd.to_reg(NEXP * 128 - 1)

            def stW(b):
                k3 = b % NW
                off = bass.IndirectOffsetOnAxis(ap=IDXW[:, b:b + 1], axis=0)
                for wt_, wv_, wn_ in ((wgb[k3], wgv, "wgb%d" % k3), (wub[k3], wuv, "wub%d" % k3), (wdb[k3], wdv, "wdb%d" % k3)):
                    DMA(lambda e: e.indirect_dma_start(out=wt_[:].rearrange("p a b -> p (a b)"), out_offset=None, in_=wv_, in_offset=off,
                                                       bounds_check=bc_reg, oob_is_err=False),
                        r=["IDXW"], w=[wn_], q="pool")

            def stX(b):
                X_, Xn = Xb[b % NX], "Xb%d" % (b % NX)
                DMA(lambda e: e.dma_start(out=X_[:], in_=R["XSs"][b * BR:(b + 1) * BR, :].rearrange("(r p) f -> p r f", p=128)), r=["XSs"], w=[Xn])

            def stT(b):
                k2 = b % 2
                X_, Xn = Xb[b % NX], "Xb%d" % (b % NX)
                for rt_ in range(2):
                    for kc in range(8):
                        PE(lambda e: e.transpose(xTp[:, kc, rt_ * 128:(rt_ + 1) * 128], X_[:, rt_, kc:D:8], identb[:]), r=[Xn, "identb"], w=["xTp"])
                A(lambda e: e.activation(out=xT[k2][:, 0:4, :], in_=xTp[:, 0:4, :], func=AF.Copy), r=["xTp"], w=["xT%d" % k2])
                V(lambda e: e.tensor_copy(out=xT[k2][:, 4:8, :], in_=xTp[:, 4:8, :]), r=["xTp"], w=["xT%d" % k2])

            def stGU(b):
                k2, k3 = b % 2, b % NW
                for fc in range(2):
                    for gu, wt_, wn_ in ((0, wgb[k3], "wgb%d" % k3), (1, wub[k3], "wub%d" % k3)):
                        for kc in range(8):
                            PE(lambda e: e.matmul(hps[:, fc * 2 + gu, :], lhsT=wt_[:, kc, fc:256:2], rhs=xT[k2][:, kc, :], start=(kc == 0), stop=(kc == 7)),
                               r=[wn_, "xT%d" % k2], w=["hps"], sig=(kc == 7 and fc == 1 and gu == 1))
                A(lambda e: e.activation(out=sg4[:], in_=hps[:, 0:4:2, :], func=AF.Silu), r=["hps"], w=["sg4"])
                V(lambda e: e.tensor_tensor(out=hT4[k2][:], in0=hps[:, 1:4:2, :], in1=sg4[:], op=ALU.mult), r=["hps", "sg4"], w=["hT4_%d" % k2])

            def stD(b):
                k2, k3 = b % 2, b % NW
                Y_, Yn = Yb[k2], "Yb%d" % k2
                for rt_ in range(2):
                    yp, ypn = yps[rt_], "yps4_%d" % rt_
                    for nh in range(2):
                        for fc in range(2):
                            PE(lambda e: e.matmul(yp[:, nh * 512:(nh + 1) * 512], lhsT=hT4[k2][:, fc, rt_ * 128:(rt_ + 1) * 128],
                                                  rhs=wdb[k3][:, fc, nh * 512:(nh + 1) * 512], start=(fc == 0), stop=(fc == 1)),
                               r=["hT4_%d" % k2, "wdb%d" % k3], w=[ypn], sig=(fc == 1 and nh == 1))
                    A(lambda e: e.activation(out=Y_[:, rt_, 0:512], in_=yp[:, 0:512], func=AF.Copy), r=[ypn], w=[Yn + "_%d" % rt_])
                    V(lambda e: e.tensor_copy(out=Y_[:, rt_, 512:D], in_=yp[:, 512:D]), r=[ypn], w=[Yn + "_%d" % rt_])
                DMA(lambda e: e.dma_start(out=R["YSs"][b * BR:(b + 1) * BR, :].rearrange("(r p) f -> p r f", p=128), in_=Y_[:]),
                    r=[Yn + "_0", Yn + "_1"], w=["YSs_%d" % b])

            for b0 in range(min(NW, blim)):
                stW(b0)
            for b0 in range(min(NX - 1, blim)):
                stX(b0)
            stT(0)
            for b in range(blim):
                if b + NX - 1 < blim:
                    stX(b + NX - 1)
                if b + 1 < blim:
                    stT(b + 1)
                stGU(b)
                if b >= 1:
                    stD(b - 1)
                if b >= 1 and b + NW - 1 < blim:
                    stW(b + NW - 1)
            stD(blim - 1)
            S.barrier()
        if stage == 5:
            return finish(nc, S, out, dbg_out, I)

        with ExitStack() as ph:
            P_ = lambda name, shape, dt=F32: sb(name, shape, dt, ctx=ph)
            g2bc, ln2g, ln2b = P_("g2bc", [128, D]), P_("ln2g", [128, D]), P_("ln2b", [128, D])
            bcast_row(g2bc, R["ADA"][3:4, :], "g2bc")
            bcast_row(ln2g, I["ln2"][0:1, :], "ln2g")
            bcast_row(ln2b, I["ln2"][1:2, :], "ln2b")
            Gk = [P_("Gk%d" % i, [128, D], BF16) for i in range(12)]
            sht = [P_("sht%d" % i, [128, D]) for i in range(3)]
            x1r = [P_("x1r%d" % i, [128, D]) for i in range(3)]

            def ld5(ti):
                k3 = ti % 3
                DMA(lambda e: e.dma_start(out=sht[k3][:], in_=R["SHs"][ti * 128:(ti + 1) * 128, :]), r=["SHs"], w=["sht%d" % k3])
                DMA(lambda e: e.dma_start(out=x1r[k3][:], in_=R["X1"][ti * 128:(ti + 1) * 128, :]), r=["X1"], w=["x1r%d" % k3])
            ld5(0)
            ot = [P_("ot%d" % i, [128, D]) for i in range(2)]
            st6, mv, rstd = P_("st6b", [128, 2, 6]), P_("mvb", [128, 2]), P_("rstdb", [128, 1])
            gi = 0
            for ti in range(glim * 4):
                k2, k3 = ti % 2, ti % 3
                a_, an = sht[k3], "sht%d" % k3
                x_, xn_ = x1r[k3], "x1r%d" % k3
                o_, on_ = ot[k2], "ot%d" % k2
                if ti + 1 < glim * 4:
                    ld5(ti + 1)
                for k in range(8):
                    g_, gn = Gk[gi % 12], "Gk%d" % (gi % 12)
                    gi += 1
                    DMA(lambda e: e.indirect_dma_start(out=g_[:], out_offset=None, in_=R["YSs"],
                                                       in_offset=bass.IndirectOffsetOnAxis(ap=DEST[:, ti * 8 + k:ti * 8 + k + 1], axis=0)),
                        r=["YSs", "DEST"], w=[gn], q="pool")
                    V(lambda e: e.scalar_tensor_tensor(out=a_[:], in0=g_[:], scalar=GATE[:, ti * 8 + k:ti * 8 + k + 1], in1=a_[:], op0=ALU.mult, op1=ALU.add),
                      r=[gn, "GATE", an], w=[an])
                V(lambda e: e.tensor_tensor(out=a_[:], in0=a_[:], in1=g2bc[:], op=ALU.mult), r=[an, "g2bc"], w=[an])
                V(lambda e: e.scalar_tensor_tensor(out=a_[:], in0=x_[:], scalar=ALPHA, in1=a_[:], op0=ALU.mult, op1=ALU.add), r=[xn_, an], w=[an])
                layer_norm(a_, an, o_, on_, ln2g, ln2b, "ln2g", "ln2b", st6, mv, rstd)
                DMA(lambda e: e.dma_start(out=out[ti * 128:(ti + 1) * 128, :], in_=o_[:]), r=[on_], w=["out"])
            S.barrier()
        return finish(nc, S, out, dbg_out, I)


def finish(nc, S, out, dbg_out, I):
    S.barrier()
    return nc, dbg_out, list(I.keys())


_SHAPES = None


def kernel(**inputs):
    maps = host_prep(inputs)
    shapes = {k: v.shape for k, v in maps[0].items()}
    nc, _, used = build(shapes)
    maps = [{k: m[k] for k in used} for m in maps]
    res = run_bass_kernel_spmd(nc, maps, core_ids=list(range(NCORES)))
    outp = np.zeros((4, SEQ, D), np.float32)
    for ci in range(NCORES):
        b, half = ci // 2, ci % 2
        outp[b, half * OWN:(half + 1) * OWN] = res.results[ci]["out"]
    return outp
```
